# Optimizing a Trainium2 kernel written in Bass

```python
import jax, jax.numpy as jnp
from jax import lax
import numpy as np

D_MODEL = 1024
BATCH = 2
SEQ = 8192
DEPTH = 4

D_MIX = 2 * D_MODEL
SSD_HEADS = 16
SSD_INNER = D_MIX // 2
SSD_HEAD_DIM = SSD_INNER // SSD_HEADS
SSD_GROUPS = 2
SSD_STATE = 128
SSD_XBC = SSD_INNER + 2 * SSD_GROUPS * SSD_STATE
SSD_CHUNK = 128
HG_HEADS = 4
HG_WIDTH = D_MIX // 4
HG_DK = HG_WIDTH // HG_HEADS
HG_CHUNK = 64
ML_HEADS = 4
ML_WIDTH = D_MIX // 4
ML_DH = ML_WIDTH // ML_HEADS
ML_CHUNK = 64
CONV_WIDTH = 5
D_FF_DENSE = 256 * ((8 * D_MODEL // 3 + 255) // 256)
N_EXPERTS = 8
TOP_K = 2
D_FF_EXPERT = 7 * D_MODEL // 2
MOE_BLOCK = 256
LN_EPS = 1e-5
RMS_EPS = 1e-6
MASK_NEG = -1e4
PROJ_SIZES = (SSD_INNER, SSD_XBC, 2 * SSD_HEADS,
              HG_WIDTH, 2 * HG_WIDTH, HG_WIDTH, HG_WIDTH,
              2 * ML_WIDTH, ML_WIDTH, ML_WIDTH, 2 * ML_HEADS, 2 * ML_HEADS)
D_PROJ = sum(PROJ_SIZES)

kernel_name = "bidir_hybrid_ssd_hgrn2_mlstm_moe"


def _split_points():
    pts, acc = [], 0
    for s in PROJ_SIZES[:-1]:
        acc += s
        pts.append(acc)
    return pts


def _flip(t):
    return jnp.flip(t, axis=1)


def layer_norm(x, g, b):
    xf = x.astype(jnp.float32)
    mu = jnp.mean(xf, -1, keepdims=True)
    xc = xf - mu
    var = jnp.mean(jnp.square(xc), -1, keepdims=True)
    return (xc * lax.rsqrt(var + LN_EPS) * g.astype(jnp.float32) + b.astype(jnp.float32)).astype(x.dtype)


def group_rms_norm(x, w, n_groups):
    shp = x.shape
    xg = x.reshape(shp[:-1] + (n_groups, shp[-1] // n_groups))
    xg = xg * lax.rsqrt(jnp.mean(jnp.square(xg), -1, keepdims=True) + RMS_EPS)
    return xg.reshape(shp) * w


def group_layer_norm(x, w, n_groups):
    shp = x.shape
    xg = x.reshape(shp[:-1] + (n_groups, shp[-1] // n_groups))
    xc = xg - jnp.mean(xg, -1, keepdims=True)
    xg = xc * lax.rsqrt(jnp.mean(jnp.square(xc), -1, keepdims=True) + LN_EPS)
    return xg.reshape(shp) * w


def centred_depthwise_conv(x, w, b):
    pad = w.shape[0] // 2
    y = lax.conv_general_dilated(x, w[:, None, :], window_strides=(1,), padding=[(pad, pad)],
                                 dimension_numbers=('NWC', 'WIO', 'NWC'),
                                 feature_group_count=x.shape[-1])
    return y + b


def ssd_scan(xh, dt, A, Bm, Cm):
    b, L, H, P = xh.shape
    G, N = Bm.shape[-2:]
    R = H // G
    nc = L // SSD_CHUNK
    X = (xh * dt[..., None]).reshape(b, nc, SSD_CHUNK, G, R, P)
    a = (dt * A).reshape(b, nc, SSD_CHUNK, G, R)
    Bc = Bm.reshape(b, nc, SSD_CHUNK, G, N)
    Cc = Cm.reshape(b, nc, SSD_CHUNK, G, N)
    a_cs = jnp.cumsum(a, axis=2)
    acs_t = jnp.moveaxis(a_cs, 2, -1)
    seg = acs_t[..., :, None] - acs_t[..., None, :]
    tri = jnp.tril(jnp.ones((SSD_CHUNK, SSD_CHUNK), bool))
    decay = jnp.exp(jnp.where(tri, seg, MASK_NEG))
    cb = jnp.einsum('bclgn,bcsgn->bcgls', Cc, Bc)
    y_diag = jnp.einsum('bcgrls,bcsgrp->bclgrp', cb[:, :, :, None] * decay, X)
    x_to_end = X * jnp.exp(a_cs[:, :, -1:] - a_cs)[..., None]
    chunk_states = jnp.einsum('bclgn,bclgrp->bcgrpn', Bc, x_to_end)
    chunk_decay = jnp.exp(a_cs[:, :, -1])

    def step(state, inp):
        st, dec = inp
        return state * dec[..., None, None] + st, state

    init = jnp.zeros((b, G, R, P, N), X.dtype)
    _, prev = lax.scan(step, init, (jnp.moveaxis(chunk_states, 1, 0), jnp.moveaxis(chunk_decay, 1, 0)))
    prev = jnp.moveaxis(prev, 0, 1)
    y_off = jnp.einsum('bclgn,bcgrpn->bclgrp', Cc, prev) * jnp.exp(a_cs)[..., None]
    return (y_diag + y_off).reshape(b, L, H, P)


def ssd_mixer(z, xbc, dt_raw, conv_w, conv_b, dt_bias, a_log, d_skip, norm_w):
    xbc = jax.nn.silu(centred_depthwise_conv(xbc, conv_w, conv_b))
    xs, Bm, Cm = jnp.split(xbc, [SSD_INNER, SSD_INNER + SSD_GROUPS * SSD_STATE], axis=-1)
    b, L, _ = xs.shape
    xh = xs.reshape(b, L, SSD_HEADS, SSD_HEAD_DIM)
    Bm = Bm.reshape(b, L, SSD_GROUPS, SSD_STATE)
    Cm = Cm.reshape(b, L, SSD_GROUPS, SSD_STATE)
    dt = jax.nn.softplus(dt_raw.reshape(b, L, 2, SSD_HEADS) + dt_bias)
    A = -jnp.exp(a_log)
    y_f = ssd_scan(xh, dt[:, :, 0], A[0], Bm, Cm)
    y_b = _flip(ssd_scan(_flip(xh), _flip(dt[:, :, 1]), A[1], _flip(Bm), _flip(Cm)))
    y = (y_f + y_b + xh * d_skip[:, None]).reshape(b, L, SSD_INNER)
    return group_rms_norm(y * jax.nn.silu(z), norm_w, SSD_GROUPS)


def hgrn2_scan(q, k, v, g):
    b, L, H, K = q.shape
    V = v.shape[-1]
    nc = L // HG_CHUNK

    def to_chunks(t):
        return t.reshape(b, nc, HG_CHUNK, H, t.shape[-1]).transpose(1, 0, 3, 2, 4)

    tri = jnp.tril(jnp.ones((HG_CHUNK, HG_CHUNK), bool))

    def step(S, inp):
        qc, kc, vc, gc = inp
        G = jnp.cumsum(gc, axis=2)
        rel = G[:, :, :, None, :] - G[:, :, None, :, :]
        rel = jnp.exp(jnp.where(tri[:, :, None], rel, MASK_NEG))
        att = jnp.einsum('bhtk,bhsk,bhtsk->bhts', qc, kc, rel)
        o = jnp.einsum('bhts,bhsv->bhtv', att, vc) + jnp.einsum('bhtk,bhkv->bhtv', qc * jnp.exp(G), S)
        G_last = G[:, :, -1:]
        S = S * jnp.exp(G_last[:, :, 0])[..., None] + jnp.einsum('bhsk,bhsv->bhkv', kc * jnp.exp(G_last - G), vc)
        return S, o

    S0 = jnp.zeros((b, H, K, V), q.dtype)
    _, o = lax.scan(step, S0, (to_chunks(q), to_chunks(k), to_chunks(v), to_chunks(g)))
    return o.transpose(1, 0, 3, 2, 4).reshape(b, L, H, V)


def hgrn2_mixer(q, f_raw, i, g_out, lb, norm_w):
    b, L, _ = q.shape
    qh = jax.nn.silu(q).reshape(b, L, HG_HEADS, HG_DK)
    vh = i.reshape(b, L, HG_HEADS, HG_DK)
    lbh = lb.reshape(HG_HEADS, HG_DK)
    fr = f_raw.reshape(b, L, 2, HG_HEADS, HG_DK)

    def gates(f):
        key = (1.0 - lbh) * jax.nn.sigmoid(-f)
        log_f = jnp.log1p(-key)
        return key, log_f

    k_f, g_f = gates(fr[:, :, 0])
    k_b, g_b = gates(fr[:, :, 1])
    o_f = hgrn2_scan(qh, k_f, vh, g_f)
    o_b = _flip(hgrn2_scan(_flip(qh), _flip(k_b), _flip(vh), _flip(g_b)))
    o = (o_f + o_b).reshape(b, L, HG_WIDTH)
    return group_rms_norm(o, norm_w, HG_HEADS) * jax.nn.silu(g_out)


def mlstm_scan(q, k, v, log_i, log_f):
    b, L, H, D = q.shape
    nc = L // ML_CHUNK

    def to_chunks(t):
        return t.reshape(b, nc, ML_CHUNK, H, D).transpose(1, 0, 3, 2, 4)

    def gate_chunks(t):
        return t.reshape(b, nc, ML_CHUNK, H).transpose(1, 0, 3, 2)

    tri = jnp.tril(jnp.ones((ML_CHUNK, ML_CHUNK), bool))

    def step(carry, inp):
        C, n, m = carry
        qc, kc, vc, ic, fc = inp
        bcum = jnp.cumsum(fc, axis=-1)
        dmat = jnp.where(tri, bcum[..., :, None] - bcum[..., None, :] + ic[..., None, :], MASK_NEG)
        a_inter = bcum + m[..., None]
        m_t = jnp.maximum(jnp.max(dmat, -1), a_inter)
        w_intra = jnp.exp(jnp.where(tri, dmat - m_t[..., None], MASK_NEG))
        w_inter = jnp.exp(a_inter - m_t)
        s = jnp.einsum('bhtd,bhsd->bhts', qc, kc) * w_intra
        num = jnp.einsum('bhts,bhsd->bhtd', s, vc) + w_inter[..., None] * jnp.einsum('bhtk,bhkv->bhtv', qc, C)
        den = jnp.sum(s, -1) + w_inter * jnp.einsum('bhtk,bhk->bht', qc, n)
        h = num / jnp.maximum(jnp.abs(den), jnp.exp(-m_t))[..., None]
        b_last = bcum[..., -1]
        log_w = b_last[..., None] - bcum + ic
        m_new = jnp.maximum(b_last + m, jnp.max(log_w, -1))
        w_s = jnp.exp(log_w - m_new[..., None])
        dec = jnp.exp(b_last + m - m_new)
        C = dec[..., None, None] * C + jnp.einsum('bhsk,bhsv->bhkv', kc * w_s[..., None], vc)
        n = dec[..., None] * n + jnp.einsum('bhsk,bhs->bhk', kc, w_s)
        return (C, n, m_new), h

    init = (jnp.zeros((b, H, D, D), q.dtype), jnp.zeros((b, H, D), q.dtype), jnp.zeros((b, H), q.dtype))
    _, h = lax.scan(step, init, (to_chunks(q), to_chunks(k), to_chunks(v), gate_chunks(log_i), gate_chunks(log_f)))
    return h.transpose(1, 0, 3, 2, 4).reshape(b, L, H, D)


def mlstm_mixer(qk, v, o_raw, ig_raw, fg_raw, conv_w, conv_b, ig_bias, fg_bias, norm_w):
    b, L, _ = v.shape
    qk = jax.nn.silu(centred_depthwise_conv(qk, conv_w, conv_b))
    q, k = jnp.split(qk, 2, axis=-1)
    qh = q.reshape(b, L, ML_HEADS, ML_DH) * (ML_DH ** -0.5)
    kh = k.reshape(b, L, ML_HEADS, ML_DH)
    vh = v.reshape(b, L, ML_HEADS, ML_DH)
    log_i = ig_raw.reshape(b, L, 2, ML_HEADS) + ig_bias
    log_f = jax.nn.log_sigmoid(fg_raw.reshape(b, L, 2, ML_HEADS) + fg_bias)
    h_f = mlstm_scan(qh, kh, vh, log_i[:, :, 0], log_f[:, :, 0])
    h_b = _flip(mlstm_scan(_flip(qh), _flip(kh), _flip(vh), _flip(log_i[:, :, 1]), _flip(log_f[:, :, 1])))
    h = (h_f + h_b).reshape(b, L, ML_WIDTH)
    return group_layer_norm(h, norm_w, ML_HEADS) * jax.nn.sigmoid(o_raw)


def swiglu(x, w1, w3, w2):
    return jnp.dot(jax.nn.silu(jnp.dot(x, w1)) * jnp.dot(x, w3), w2)


def moe_swiglu(x, router_w, w1, w3, w2):
    b, L, D = x.shape
    T = b * L
    xf = x.reshape(T, D)
    logits = jnp.dot(xf, router_w).astype(jnp.float32)
    top_vals, top_idx = lax.top_k(logits, TOP_K)
    gates = jax.nn.softmax(top_vals, axis=-1).astype(x.dtype)
    expert_ids = top_idx.reshape(-1).astype(jnp.int32)
    token_ids = jnp.repeat(jnp.arange(T, dtype=jnp.int32), TOP_K)
    flat_gates = gates.reshape(-1)
    order = jnp.argsort(expert_ids)
    sorted_e = expert_ids[order]
    counts = jnp.zeros((N_EXPERTS,), jnp.int32).at[expert_ids].add(1)
    padded = (counts + MOE_BLOCK - 1) // MOE_BLOCK * MOE_BLOCK
    start = jnp.cumsum(counts) - counts
    ends = jnp.cumsum(padded)
    pstart = ends - padded
    rank = jnp.arange(T * TOP_K, dtype=jnp.int32) - start[sorted_e]
    dest = pstart[sorted_e] + rank
    n_blocks = -(-(T * TOP_K) // MOE_BLOCK) + N_EXPERTS
    n_rows = n_blocks * MOE_BLOCK
    row_token = jnp.zeros((n_rows,), jnp.int32).at[dest].set(token_ids[order])
    row_gate = jnp.zeros((n_rows,), x.dtype).at[dest].set(flat_gates[order])
    block_start = jnp.arange(n_blocks, dtype=jnp.int32) * MOE_BLOCK
    block_expert = jnp.minimum(jnp.searchsorted(ends, block_start, side='right'), N_EXPERTS - 1)
    xs = xf[row_token].reshape(n_blocks, MOE_BLOCK, D)

    def expert_block(args):
        xb, e = args
        return jnp.dot(jax.nn.silu(jnp.dot(xb, w1[e])) * jnp.dot(xb, w3[e]), w2[e])

    ys = lax.map(expert_block, (xs, block_expert)).reshape(n_rows, D)
    out = jnp.zeros((T, D), x.dtype).at[row_token].add(ys * row_gate[:, None])
    return out.reshape(b, L, D)


def setup_inputs(seed: int = 0) -> dict:
    key = jax.random.key(seed)
    ks = jax.random.split(key, 40)
    f32 = jnp.float32
    nrm = lambda k, shape, scale: jax.random.normal(k, shape, f32) * scale
    beta = (8.0 * DEPTH) ** -0.25
    n_dense = (DEPTH + 1) // 2
    n_moe = DEPTH // 2
    dt0 = jnp.exp(jax.random.uniform(ks[6], (DEPTH, 2, SSD_HEADS), f32, np.log(1e-3), np.log(1e-1)))
    fg_base = jnp.linspace(3.0, 6.0, ML_HEADS, dtype=f32)
    return {
        'x': nrm(ks[0], (BATCH, SEQ, D_MODEL), 1.0),
        'ln_in_g': 1.0 + nrm(ks[1], (D_MODEL,), 0.02),
        'ln_in_b': nrm(ks[2], (D_MODEL,), 0.02),
        'w_in': nrm(ks[3], (DEPTH, D_MODEL, D_PROJ), D_MODEL ** -0.5),
        'ssd_conv_w': nrm(ks[4], (DEPTH, CONV_WIDTH, SSD_XBC), CONV_WIDTH ** -0.5),
        'ssd_conv_b': nrm(ks[5], (DEPTH, SSD_XBC), 0.02),
        'ssd_dt_bias': dt0 + jnp.log(-jnp.expm1(-dt0)),
        'ssd_a_log': jnp.log(jax.random.uniform(ks[7], (DEPTH, 2, SSD_HEADS), f32, 1.0, 16.0)),
        'ssd_d': 1.0 + nrm(ks[8], (DEPTH, SSD_HEADS), 0.1),
        'ssd_norm_w': 1.0 + nrm(ks[9], (DEPTH, SSD_INNER), 0.02),
        'hg_lb_logits': nrm(ks[10], (DEPTH, HG_WIDTH), 0.1),
        'hg_norm_w': 1.0 + nrm(ks[11], (DEPTH, HG_WIDTH), 0.02),
        'ml_conv_w': nrm(ks[12], (DEPTH, CONV_WIDTH, 2 * ML_WIDTH), CONV_WIDTH ** -0.5),
        'ml_conv_b': nrm(ks[13], (DEPTH, 2 * ML_WIDTH), 0.02),
        'ml_ig_bias': nrm(ks[14], (DEPTH, 2, ML_HEADS), 0.1),
        'ml_fg_bias': fg_base + nrm(ks[15], (DEPTH, 2, ML_HEADS), 0.1),
        'ml_norm_w': 1.0 + nrm(ks[16], (DEPTH, ML_WIDTH), 0.02),
        'w_out': nrm(ks[17], (DEPTH, D_MIX, D_MODEL), D_MIX ** -0.5 * beta),
        'ln1_g': 1.0 + nrm(ks[18], (DEPTH, D_MODEL), 0.02),
        'ln1_b': nrm(ks[19], (DEPTH, D_MODEL), 0.02),
        'ln2_g': 1.0 + nrm(ks[20], (DEPTH, D_MODEL), 0.02),
        'ln2_b': nrm(ks[21], (DEPTH, D_MODEL), 0.02),
        'ffn_w1': nrm(ks[22], (n_dense, D_MODEL, D_FF_DENSE), D_MODEL ** -0.5),
        'ffn_w3': nrm(ks[23], (n_dense, D_MODEL, D_FF_DENSE), D_MODEL ** -0.5),
        'ffn_w2': nrm(ks[24], (n_dense, D_FF_DENSE, D_MODEL), D_FF_DENSE ** -0.5 * beta),
        'moe_router': nrm(ks[25], (n_moe, D_MODEL, N_EXPERTS), D_MODEL ** -0.5),
        'moe_w1': nrm(ks[26], (n_moe, N_EXPERTS, D_MODEL, D_FF_EXPERT), D_MODEL ** -0.5),
        'moe_w3': nrm(ks[27], (n_moe, N_EXPERTS, D_MODEL, D_FF_EXPERT), D_MODEL ** -0.5),
        'moe_w2': nrm(ks[28], (n_moe, N_EXPERTS, D_FF_EXPERT, D_MODEL), D_FF_EXPERT ** -0.5 * beta),
    }


def reference(x, ln_in_g, ln_in_b, w_in, ssd_conv_w, ssd_conv_b, ssd_dt_bias, ssd_a_log, ssd_d,
              ssd_norm_w, hg_lb_logits, hg_norm_w, ml_conv_w, ml_conv_b, ml_ig_bias, ml_fg_bias,
              ml_norm_w, w_out, ln1_g, ln1_b, ln2_g, ln2_b, ffn_w1, ffn_w3, ffn_w2,
              moe_router, moe_w1, moe_w3, moe_w2):
    f32 = lambda t: t.astype(jnp.float32)
    alpha = (2.0 * DEPTH) ** 0.25
    lb_soft = jax.nn.softmax(f32(hg_lb_logits), axis=0)
    lb_all = jnp.cumsum(lb_soft, axis=0) - lb_soft[0]
    pts = _split_points()
    h = layer_norm(x, ln_in_g, ln_in_b)
    for l in range(DEPTH):
        proj = f32(jnp.dot(h, w_in[l]))
        (z, xbc, dt_raw, hq, hf, hi, hg, mqk, mv, mo, mig, mfg) = jnp.split(proj, pts, axis=-1)
        y_ssd = ssd_mixer(z, xbc, dt_raw, f32(ssd_conv_w[l]), f32(ssd_conv_b[l]), f32(ssd_dt_bias[l]),
                          f32(ssd_a_log[l]), f32(ssd_d[l]), f32(ssd_norm_w[l]))
        y_hg = hgrn2_mixer(hq, hf, hi, hg, lb_all[l], f32(hg_norm_w[l]))
        y_ml = mlstm_mixer(mqk, mv, mo, mig, mfg, f32(ml_conv_w[l]), f32(ml_conv_b[l]),
                           f32(ml_ig_bias[l]), f32(ml_fg_bias[l]), f32(ml_norm_w[l]))
        mix = jnp.concatenate([y_ssd, y_hg, y_ml], axis=-1).astype(h.dtype)
        h = layer_norm(alpha * h + jnp.dot(mix, w_out[l]), ln1_g[l], ln1_b[l])
        if l % 2 == 0:
            f = swiglu(h, ffn_w1[l // 2], ffn_w3[l // 2], ffn_w2[l // 2])
        else:
            f = moe_swiglu(h, moe_router[l // 2], moe_w1[l // 2], moe_w3[l // 2], moe_w2[l // 2])
        h = layer_norm(alpha * h + f, ln2_g[l], ln2_b[l])
    return h
```

```python
import re
import numpy as np
import concourse.bass as bass
import concourse.mybir as mybir
from concourse.bass_utils import run_bass_kernel_spmd

F32 = mybir.dt.float32
BF16 = mybir.dt.bfloat16
ALU = mybir.AluOpType
AF = mybir.ActivationFunctionType
AX = mybir.AxisListType

NCORES = 8
D = 1024
T = 2048
NCH = 16
DEPTH = 4
DPROJ = 7216
NDMA = 6


class Sched:
    ENG = ("pe", "act", "dve", "pool", "sp")

    def __init__(self, nc):
        self.nc = nc
        self.q = {e: [] for e in self.ENG}
        self.cnt = {}
        self.lastw = {}
        self.readers = {}
        self.seen = {e: {} for e in self.ENG}
        self.host = {"pe": "pe", "act": "act", "dve": "dve", "pool": "pool", "sp": "sp", "cc": "pool"}
        for j in range(NDMA):
            self.host["dsp%d" % j] = "sp"
            self.host["dpool%d" % j] = "pool"
        self.dcount = {}

    def _dep(self, unit, R, W):
        waits = {}

        def need(u, i):
            if i > waits.get(u, 0):
                waits[u] = i
        for b in R:
            lw = self.lastw.get(b)
            if lw:
                need(*lw)
        for b in W:
            lw = self.lastw.get(b)
            if lw:
                need(*lw)
            for u, i in self.readers.get(b, {}).items():
                need(u, i)
        return waits

    @staticmethod
    def canon(keys):
        out = []
        for k in keys:
            if isinstance(k, str):
                m = re.match(r"^(wt|wb)(\d+)", k)
                if m:
                    k = m.group(1) + m.group(2)
            out.append(k)
        return tuple(out)

    def op(self, unit, fn, R=(), W=()):
        R = self.canon(R)
        W = self.canon(W)
        if unit in ("dsp", "dpool"):
            n = self.dcount.get(unit, 0)
            self.dcount[unit] = n + 1
            unit = "%s%d" % (unit, n % NDMA)
        host = self.host[unit]
        waits = self._dep(unit, R, W)
        idx = self.cnt.get(unit, 0) + 1
        self.cnt[unit] = idx
        if unit[0] == "d" and idx > 1:
            waits[unit] = max(waits.get(unit, 0), idx - 1)
        if unit == "pe":
            waits.pop("pe", None)
        wl = []
        for u, i in waits.items():
            if self.seen[host].get(u, 0) >= i:
                continue
            self.seen[host][u] = i
            wl.append((u, i))
        self.q[host].append((wl, fn, unit, idx))
        for b in W:
            self.lastw[b] = (unit, idx)
            self.readers[b] = {}
        for b in R:
            self.readers.setdefault(b, {})[unit] = idx
        return (unit, idx)

    def emit(self, final_waits):
        nc = self.nc
        import contextlib
        EP = 30000
        EPD = 1800
        dunits = ["dsp%d" % j for j in range(NDMA)] + ["dpool%d" % j for j in range(NDMA)]
        with contextlib.ExitStack() as st:
            sems = {}
            for u in ["pe", "act", "dve", "pool", "sp", "cc"] + dunits:
                ep = EPD if u in dunits else EP
                n = self.cnt.get(u, 0) // ep + 1
                sems[u] = [st.enter_context(nc.semaphore("s_%s_%d" % (u, j))) for j in range(n)]
            block = st.enter_context(nc.Block())

            def semval(u, i):
                if u in dunits:
                    return sems[u][(i - 1) // EPD], 16 * ((i - 1) % EPD + 1)
                return sems[u][(i - 1) // EP], (i - 1) % EP + 1

            def run(hostname, eng):
                for wl, fn, unit, idx in self.q[hostname]:
                    for u, i in wl:
                        s_, v = semval(u, i)
                        eng.wait_ge(s_, v)
                    ins = fn(eng)
                    s_, v = semval(unit, idx)
                    ins.then_inc(s_, 16 if unit in dunits else 1)
                if hostname == "sp":
                    for u, i in final_waits:
                        s_, v = semval(u, i)
                        eng.wait_ge(s_, v)

            block.tensor(lambda e: run("pe", e))
            block.scalar(lambda e: run("act", e))
            block.vector(lambda e: run("dve", e))
            block.gpsimd(lambda e: run("pool", e))
            block.sync(lambda e: run("sp", e))


class KB:
    def __init__(self, nc):
        self.nc = nc
        self.s = Sched(nc)
        self._sb = []

    def sb(self, name, shape, dt=F32):
        return self.nc.alloc_sbuf_tensor("sb_" + name, list(shape), dt)

    def ps(self, name, shape, dt=F32):
        return self.nc.alloc_psum_tensor("pp_" + name, list(shape), dt)

    def dram(self, name, shape, dt=F32):
        return self.nc.dram_tensor("d_" + name, list(shape), dt).ap()

    def mm(self, out, lhsT, rhs, start, stop, R, W):
        return self.s.op("pe", lambda e: e.matmul(out, lhsT, rhs, start=start, stop=stop, skip_group_check=True), R, W)

    def tr(self, out, in_, ident, R, W):
        return self.s.op("pe", lambda e: e.transpose(out, in_, ident), R, W)

    def act(self, out, in_, func, R, W, bias=None, scale=1.0):
        if bias is None:
            return self.s.op("act", lambda e: e.activation(out, in_, func, scale=scale), R, W)
        return self.s.op("act", lambda e: e.activation(out, in_, func, bias=bias, scale=scale), R, W)

    def tt(self, out, in0, in1, op, R, W, eng="dve"):
        return self.s.op(eng, lambda e: e.tensor_tensor(out, in0, in1, op), R, W)

    def ts(self, out, in0, s1, s2, op0, op1, R, W, eng="dve"):
        if s2 is None:
            return self.s.op(eng, lambda e: e.tensor_scalar(out, in0, s1, None, op0), R, W)
        return self.s.op(eng, lambda e: e.tensor_scalar(out, in0, s1, s2, op0, op1), R, W)

    def stt(self, out, in0, scalar, in1, op0, op1, R, W, eng="dve"):
        return self.s.op(eng, lambda e: e.scalar_tensor_tensor(out, in0, scalar, in1, op0, op1), R, W)

    def copy(self, out, in_, R, W, eng="dve"):
        if eng == "act":
            return self.s.op(eng, lambda e: e.copy(out, in_), R, W)
        return self.s.op(eng, lambda e: e.tensor_copy(out, in_), R, W)

    def memset(self, ap, val, W, eng="dve"):
        return self.s.op(eng, lambda e: e.memset(ap, val), (), W)

    def red(self, out, in_, op, R, W, eng="dve"):
        return self.s.op(eng, lambda e: e.tensor_reduce(out, in_, AX.X, op), R, W)

    def recip(self, out, in_, R, W):
        return self.s.op("dve", lambda e: e.reciprocal(out, in_), R, W)

    def dma(self, out, in_, R, W, q="dsp"):
        return self.s.op(q, lambda e: e.dma_start(out=out, in_=in_), R, W)

    def dmac(self, out, in_, R, W):
        return self.s.op("dpool", lambda e: e.dma_start(out=out, in_=in_), R, W)

    def dmac3(self, out3, in3, n, R, W):
        r = None
        for j in range(n):
            r = self.dmac(out3[:, j, :], in3[:, j, :], R, W)
        return r

    def allgather(self, out, in_, R, W):
        return self.s.op("cc", lambda e: e.collective_compute(
            "AllGather", ALU.bypass, replica_groups=[list(range(NCORES))], ins=[in_], outs=[out]), R, W)


PP_OFF = {}
_o = 0
for _n, _w in (("ln1_g", 1024), ("ln1_b", 1024), ("ln2_g", 1024), ("ln2_b", 1024), ("ssd_nw", 1024),
               ("dskip", 16), ("hg_nw", 512), ("ml_nw", 512), ("dt_bias", 32), ("a_log", 32),
               ("ig_bias", 8), ("fg_bias", 8)):
    PP_OFF[_n] = (_o, _w)
    _o += _w
NPP = _o
NCONST = 9
C_Z, C_XBC, C_DT, C_HQ, C_HF, C_HI, C_HG, C_MQK, C_MV, C_MO, C_MIG, C_MFG = (
    0, 1024, 2560, 2592, 3104, 4128, 4640, 5152, 6176, 6688, 7200, 7208)


def make_consts():
    s = np.arange(128)[:, None]
    l = np.arange(128)[None, :]
    same = (s // 64) == (l // 64)
    c = np.zeros((128, NCONST, 128), np.float32)
    c[:, 0] = (s == l)
    c[:, 1] = 1.0
    c[:, 2] = (s <= l)
    c[:, 3] = (s >= l)
    c[:, 4] = np.where(s <= l, 0.0, -30000.0)
    c[:, 5] = np.where(s >= l, 0.0, -30000.0)
    c[:, 6] = (s <= l) & same
    c[:, 7] = (s >= l) & same
    c[:, 8] = same
    return c


def build(L_RUN=DEPTH):
    nc = bass.Bass("TRN2", target_bir_lowering=False)
    kb = KB(nc)

    def inp(name, shape):
        return nc.dram_tensor(name, list(shape), F32, kind="ExternalInput").ap()

    x_in = inp("x", [T, D])
    sel_in = inp("sel", [32, 4])
    consts_in = inp("consts", [128, NCONST, 128])
    lnin_in = inp("lnin", [128, 2, 1024])
    pp_in = inp("pp", [DEPTH, 128, NPP])
    pc_in = inp("pc", [DEPTH, 128, 120])
    lbtm_in = inp("lbtm", [128, DEPTH, 512])
    lbfm_in = inp("lbfm", [128, DEPTH, 4])
    win_s = inp("win_s", [DEPTH, 128, DPROJ])
    wout_s = inp("wout_s", [DEPTH, 256, D])
    f1_s = inp("f1_s", [2, 128, 2816])
    f3_s = inp("f3_s", [2, 128, 2816])
    f2_s = inp("f2_s", [2, 352, D])
    NMOE = (L_RUN) // 2
    if NMOE > 0:
        m1_s = inp("m1_s", [2, D, 3584])
        m3_s = inp("m3_s", [2, D, 3584])
        m2_s = inp("m2_s", [2, 3584, D])
        router_in = inp("router", [2, 128, 64])
    y_out = nc.dram_tensor("y", [T, D], F32, kind="ExternalOutput").ap()

    win_g = [kb.dram("win_g%d" % l, [D, DPROJ]) for l in range(DEPTH)]
    wout_g = [kb.dram("wout_g%d" % l, [2048, D]) for l in range(DEPTH)]
    f1_g = [kb.dram("f1_g%d" % i, [D, 2816]) for i in range(2)]
    f3_g = [kb.dram("f3_g%d" % i, [D, 2816]) for i in range(2)]
    f2_g = [kb.dram("f2_g%d" % i, [2816, D]) for i in range(2)]
    m1_g = [[kb.dram("m1_g%d_%d" % (i, k), [8 * 128, 3584]) for k in range(8)] for i in range(NMOE)]
    m3_g = [[kb.dram("m3_g%d_%d" % (i, k), [8 * 128, 3584]) for k in range(8)] for i in range(NMOE)]
    m2_g = [[kb.dram("m2_g%d_%d" % (i, j), [8 * 512, D]) for j in range(7)] for i in range(NMOE)]

    _gcount = [0]

    def gather(dst, src, key):
        _gcount[0] += 1
        stg = kb.dram("stg%d" % _gcount[0], list(src.shape))
        kb.dma(stg[:, :], src, (), (("stg",) + key,))
        kb.allgather(dst, stg[:, :], (("stg",) + key,), (key,))

    def gather_layer(l):
        gather(win_g[l][:, :], win_s[l], ("win", l))
        gather(wout_g[l][:, :], wout_s[l], ("wout", l))
        i = l // 2
        if l % 2 == 0:
            gather(f1_g[i][:, :], f1_s[i], ("f1", i))
            gather(f3_g[i][:, :], f3_s[i], ("f3", i))
            gather(f2_g[i][:, :], f2_s[i], ("f2", i))
        else:
            for k in range(8):
                gather(m1_g[i][k][:, :], m1_s[i][k * 128:(k + 1) * 128, :], ("m1", i))
                gather(m3_g[i][k][:, :], m3_s[i][k * 128:(k + 1) * 128, :], ("m3", i))
            for j in range(7):
                gather(m2_g[i][j][:, :], m2_s[i][j * 512:(j + 1) * 512, :], ("m2", i))

    h_d = kb.dram("h_d", [T, D])
    edge_d = kb.dram("edge_d", [4, D])
    edges_all = kb.dram("edges_all", [32, D])
    xs_tm = kb.dram("xs_tm", [T, 1024])
    b_tm = kb.dram("b_tm", [T, 256])
    bc_fm = kb.dram("bc_fm", [512, T])
    mqk_fm = kb.dram("mqk_fm", [1024, T])
    mk_tm = kb.dram("mk_tm", [T, 512])
    hq_fm = kb.dram("hq_fm", [512, T])
    key_fm = kb.dram("key_fm", [1024, T])
    key_tm = kb.dram("key_tm", [T, 1024])
    g_tm = kb.dram("g_tm", [T, 1024])
    zs_tm = kb.dram("zs_tm", [T, 1024])
    hi_tm = kb.dram("hi_tm", [T, 512])
    hg_tm = kb.dram("hg_tm", [T, 512])
    mv_tm = kb.dram("mv_tm", [T, 512])
    mo_tm = kb.dram("mo_tm", [T, 512])
    gat_tm = kb.dram("gat_tm", [T, 80])
    yssd_d = kb.dram("yssd_d", [T, 1024])
    hgo_d = kb.dram("hgo_d", [T, 512])
    mln_d = kb.dram("mln_d", [T, 2 * 4 * 129])
    ussd_d = kb.dram("ussd_d", [2 * NCH * 128, 1024])
    uhg_d = kb.dram("uhg_d", [2 * 2 * NCH * 128, 512])
    uml_d = kb.dram("uml_d", [2 * NCH * 128, 516])
    sum_d = kb.dram("sum_d", [128, 4160])
    sum_all = kb.dram("sum_all", [8 * 128, 4160])
    sssd_d = kb.dram("sssd_d", [2 * NCH * 128, 1024])
    shg_d = kb.dram("shg_d", [2 * 2 * NCH * 128, 512])
    sml_d = kb.dram("sml_d", [2 * NCH * 128, 516])
    facc_d = kb.dram("facc_d", [T, D])
    mix_d = kb.dram("mix_d", [T, 2048])
    h1_d = kb.dram("h1_d", [T, D])

    consts = kb.sb("consts", [128, NCONST, 128])
    constb = kb.sb("constb", [128, NCONST, 128], BF16)
    IDENT, ONES, TRIF, TRIB, NEGF, NEGB, BTF, BTB, BONES = [consts[:, i, :] for i in range(NCONST)]
    identb = constb[:, 0, :]
    hT = kb.sb("hT", [128, 8, T + 4], BF16)
    pp = kb.sb("pp", [128, NPP])
    pcs = kb.sb("pcs", [128, 120])
    oml_tm = kb.sb("oml_tm", [128, DEPTH, 512])
    oml_fm = kb.sb("oml_fm", [128, DEPTH, 4])
    sel_sb = kb.sb("sel_sb", [32, 4])
    WSET = [kb.sb("wset%d" % i, [128, 15360], BF16) for i in range(2)]
    WT = [kb.sb("wt%d" % i, [128, 1024]) for i in range(8)]
    WB = [kb.sb("wb%d" % i, [128, 1024], BF16) for i in range(6)]
    PS = [kb.ps("ps%d" % i, [128, 512]) for i in range(8)]
    small = kb.sb("small", [128, 112])
    gates_all = kb.sb("gates_all", [128, NCH, 8])
    acs_all = kb.sb("acs_all", [128, NCH, 40])
    atot_all = kb.sb("atot_all", [128, NCH, 40])

    def ppv(name):
        o, w = PP_OFF[name]
        return pp[:, o:o + w]

    kb.dma(consts[:], consts_in, (), ("consts",))
    kb.copy(constb[:], consts[:], ("consts",), ("constb",))
    kb.dma(sel_sb[:], sel_in, (), ("sel",))
    gather_layer(0)

    def compute_oml(dst, src_in, width, tag):
        lgv = [WT[4 + l][:, 0:width] for l in range(DEPTH)]
        lk = ["wt%d" % (4 + l) for l in range(DEPTH)]
        for l in range(DEPTH):
            kb.dma(lgv[l], src_in[:, l, :], (), (lk[l],))
        mx = WT[1][:, 0:width]
        kb.tt(mx, lgv[0], lgv[1], ALU.max, (lk[0], lk[1]), ("wt1",))
        kb.tt(mx, mx, lgv[2], ALU.max, (lk[2], "wt1"), ("wt1",))
        kb.tt(mx, mx, lgv[3], ALU.max, (lk[3], "wt1"), ("wt1",))
        for l in range(DEPTH):
            kb.tt(lgv[l], lgv[l], mx, ALU.subtract, (lk[l], "wt1"), (lk[l],))
            kb.act(lgv[l], lgv[l], AF.Exp, (lk[l],), (lk[l],))
        sm = WT[2][:, 0:width]
        kb.tt(sm, lgv[0], lgv[1], ALU.add, (lk[0], lk[1]), ("wt2",))
        kb.tt(sm, sm, lgv[2], ALU.add, (lk[2], "wt2"), ("wt2",))
        kb.tt(sm, sm, lgv[3], ALU.add, (lk[3], "wt2"), ("wt2",))
        kb.recip(sm, sm, ("wt2",), ("wt2",))
        cum = WT[3][:, 0:width]
        kb.memset(cum, 0.0, ("wt3",))
        for l in range(DEPTH):
            if l > 0:
                kb.tt(cum, cum, lgv[l], ALU.add, (lk[l], "wt3"), ("wt3",))
            d = dst[:, l, :]
            kb.tt(d, cum, sm, ALU.mult, ("wt3", "wt2"), (tag,))
            kb.ts(d, d, -1.0, 1.0, ALU.mult, ALU.add, (tag,), (tag,))

    compute_oml(oml_tm, lbtm_in, 512, "oml_tm")
    compute_oml(oml_fm, lbfm_in, 4, "oml_fm")
    kb.dma(WT[6][:], lnin_in[:, 0, :], (), ("wt6",))
    kb.dma(WT[7][:], lnin_in[:, 1, :], (), ("wt7",))

    def layernorm(dst, src, g_ap, b_ap, R, W):
        st = small[:, 0:12].rearrange("p (a b) -> p a b", a=2)
        kb.s.op("dve", lambda e: e.bn_stats(st[:, 0, :], src[:, 0:512]), R, ("small",))
        kb.s.op("dve", lambda e: e.bn_stats(st[:, 1, :], src[:, 512:1024]), R, ("small",))
        mv = small[:, 12:14]
        kb.s.op("dve", lambda e: e.bn_aggr(mv, st), ("small",), ("small",))
        rs = small[:, 14:15]
        kb.ts(rs, small[:, 13:14], 1e-5, None, ALU.add, None, ("small",), ("small",))
        kb.act(rs, rs, AF.Ln, ("small",), ("small",))
        kb.act(rs, rs, AF.Exp, ("small",), ("small",), scale=-0.5)
        nmr = small[:, 15:16]
        kb.tt(nmr, small[:, 12:13], rs, ALU.mult, ("small",), ("small",))
        kb.ts(nmr, nmr, -1.0, None, ALU.mult, None, ("small",), ("small",))
        kb.act(dst, src, AF.Identity, tuple(R) + ("small",), W, bias=nmr, scale=rs)
        kb.tt(dst, dst, g_ap, ALU.mult, W, W)
        kb.tt(dst, dst, b_ap, ALU.add, W, W)

    def emit_hT(c, hsrc, Rk):
        for half in range(2):
            ps = PS[6 + half]
            pk = "ps%d" % (6 + half)
            for k4 in range(4):
                k = half * 4 + k4
                kb.tr(ps[:, k4 * 128:(k4 + 1) * 128], hsrc[:, k * 128:(k + 1) * 128], IDENT, tuple(Rk) + ("consts",), (pk,))
            kb.copy(hT[:, half * 4:half * 4 + 4, 2 + c * 128:2 + (c + 1) * 128],
                    ps[:, :].rearrange("p (k t) -> p k t", k=4), (pk,), ("hT",), eng="act" if half else "dve")

    for c in range(NCH):
        xt = WT[c % 2]
        xk = "wt%d" % (c % 2)
        kb.dma(xt[:], x_in[c * 128:(c + 1) * 128, :], (), (xk,))
        ht = WT[2 + c % 2]
        hk = "wt%d" % (2 + c % 2)
        layernorm(ht[:], xt[:], WT[6][:], WT[7][:], (xk, "wt6", "wt7"), (hk,))
        kb.dma(h_d[c * 128:(c + 1) * 128, :], ht[:], (hk,), ("h_d",))
        emit_hT(c, ht, (hk,))
        if c == 0:
            kb.dma(edge_d[0:2, :], ht[0:2, :], (hk,), ("edge_d",))
        if c == NCH - 1:
            kb.dma(edge_d[2:4, :], ht[126:128, :], (hk,), ("edge_d",))


    QSCALE = 128.0 ** -0.5

    def load_layer_params(l):
        kb.dma(pp[:], pp_in[l], (), ("pp",))
        kb.dma(pcs[:], pc_in[l], (), ("pcs",))
        o, w = PP_OFF["a_log"]
        kb.act(pp[:, o:o + w], pp[:, o:o + w], AF.Exp, ("pp",), ("pp",))
        kb.ts(pp[:, o:o + w], pp[:, o:o + w], -1.0, None, ALU.mult, None, ("pp",), ("pp",))

    def halo():
        kb.allgather(edges_all[:, :], edge_d[:, :], ("edge_d",), ("edges_all",))
        eg = WT[0][0:32, :]
        kb.dma(eg, edges_all[:, :], ("edges_all",), ("wt0",))
        for half in range(2):
            ps = PS[6 + half]
            pk = "ps%d" % (6 + half)
            for k4 in range(4):
                k = half * 4 + k4
                kb.mm(ps[:, k4 * 4:(k4 + 1) * 4], eg[:, k * 128:(k + 1) * 128], sel_sb[:, :], True, True,
                      ("wt0", "sel"), (pk,))
            src = ps[:, 0:16].rearrange("p (k j) -> p k j", k=4)
            kb.copy(hT[:, half * 4:half * 4 + 4, 0:2], src[:, :, 0:2], (pk,), ("hT",))
            kb.copy(hT[:, half * 4:half * 4 + 4, T + 2:T + 4], src[:, :, 2:4], (pk,), ("hT",))

    def load_w(arena_i, wsrc, col0, n, rkey):
        ar = WSET[arena_i]
        view = ar[:, 0:8 * n].rearrange("p (k n) -> p k n", k=8)
        kb.dmac3(view, wsrc[:, col0:col0 + n].rearrange("(k p) c -> p k c", p=128), 8, (rkey,), ("wset%d" % arena_i,))
        return view

    def phase_A1(l):
        wk = ("win", l)
        groups = [(0, C_XBC, 12, 0), (1, C_MQK, 8, 12), (0, C_HQ, 12, None)]
        for (ai, col0, ntiles, cbase) in groups:
            W = load_w(ai, win_g[l], col0, ntiles * 128, wk)
            akey = "wset%d" % ai
            for tb in range(8):
                t0 = tb * 256
                for ct in range(ntiles):
                    ps = PS[ct % 2]
                    pk = "ps%d" % (ct % 2)
                    ncol = 260 if cbase is not None else 256
                    c0 = t0 if cbase is not None else t0 + 2
                    for k in range(8):
                        kb.mm(ps[:, 0:ncol], W[:, k, ct * 128:(ct + 1) * 128], hT[:, k, c0:c0 + ncol],
                              k == 0, k == 7, (akey, "hT"), (pk,))
                    o = WT[ct % 2]
                    ok = "wt%d" % (ct % 2)
                    ov = o[:, 0:256]
                    if cbase is not None:
                        t5 = (cbase + ct) * 5
                        acc = WT[2 + ct % 2][:, 0:256]
                        ak = "wt%d" % (2 + ct % 2)
                        kb.act(acc, ps[:, 0:256], AF.Copy, (pk, "pcs"), (ak,), scale=pcs[:, t5:t5 + 1])
                        for j in range(1, 5):
                            kb.stt(acc, ps[:, j:j + 256], pcs[:, t5 + j:t5 + j + 1], acc, ALU.mult, ALU.add,
                                   (pk, "pcs", ak), (ak,))
                        kb.act(ov, acc, AF.Silu, (ak, "pcs"), (ok,), bias=pcs[:, 100 + cbase + ct:101 + cbase + ct])
                    fm_dst = None
                    tm_dst = None
                    if cbase == 0:
                        if ct < 8:
                            tm_dst = (xs_tm, ct * 128, "xs_tm")
                        elif ct < 10:
                            fm_dst = (bc_fm, (ct - 8) * 128, "bc_fm")
                            tm_dst = (b_tm, (ct - 8) * 128, "b_tm")
                        else:
                            fm_dst = (bc_fm, 256 + (ct - 10) * 128, "bc_fm")
                    elif cbase == 12:
                        fm_dst = (mqk_fm, ct * 128, "mqk_fm")
                        if ct < 4:
                            kb.ts(ov, ov, QSCALE, None, ALU.mult, None, (ok,), (ok,))
                        else:
                            tm_dst = (mk_tm, (ct - 4) * 128, "mk_tm")
                    else:
                        if ct < 4:
                            kb.act(ov, ps[:, 0:256], AF.Silu, (pk,), (ok,))
                            fm_dst = (hq_fm, ct * 128, "hq_fm")
                        else:
                            kb.act(ov, ps[:, 0:256], AF.Sigmoid, (pk,), (ok,), scale=-1.0)
                            kb.ts(ov, ov, oml_fm[:, l, (ct - 4) % 4:(ct - 4) % 4 + 1], None, ALU.mult, None,
                                  (ok, "oml_fm"), (ok,))
                            fm_dst = (key_fm, (ct - 4) * 128, "key_fm")
                    if fm_dst is not None:
                        dt_, r0, dk = fm_dst
                        kb.dma(dt_[r0:r0 + 128, t0:t0 + 256], ov, (ok,), (dk,))
                    if tm_dst is not None:
                        dt_, cc0, dk = tm_dst
                        pst = PS[2 + ct % 2]
                        psk = "ps%d" % (2 + ct % 2)
                        for hf_ in range(2):
                            kb.tr(pst[:, hf_ * 128:(hf_ + 1) * 128], o[:, hf_ * 128:(hf_ + 1) * 128], IDENT,
                                  (ok, "consts"), (psk,))
                        stg = WT[4 + ct % 2][:, 0:256]
                        sk = "wt%d" % (4 + ct % 2)
                        kb.copy(stg, pst[:, 0:256], (psk,), (sk,), eng="act")
                        kb.dma(dt_[t0:t0 + 256, cc0:cc0 + 128].rearrange("(h t) c -> t h c", h=2),
                               stg.rearrange("t (h c) -> t h c", h=2), (sk,), (dk,))

    def phase_A2(l):
        wk = ("win", l)

        def run_group(ai, col0, ncols, post):
            W = load_w(ai, win_g[l], col0, ncols, wk)
            akey = "wset%d" % ai
            nblk = (ncols + 511) // 512
            for c in range(NCH):
                for b in range(nblk):
                    n = min(512, ncols - b * 512)
                    ps = PS[(c * nblk + b) % 2]
                    pk = "ps%d" % ((c * nblk + b) % 2)
                    for k in range(8):
                        kb.mm(ps[:, 0:n], hT[:, k, 2 + c * 128:2 + (c + 1) * 128], W[:, k, b * 512:b * 512 + n],
                              k == 0, k == 7, (akey, "hT"), (pk,))
                    post(c, b, ps, pk, n)

        rows = lambda c: slice(c * 128, (c + 1) * 128)

        def simple(dst, dkey, func, scale=1.0):
            def post(c, b, ps, pk, n):
                o = WT[2 + (c + b) % 2][:, 0:n]
                ok = "wt%d" % (2 + (c + b) % 2)
                kb.act(o, ps[:, 0:n], func, (pk,), (ok,), scale=scale)
                kb.dma(dst[rows(c), b * 512:b * 512 + n], o, (ok,), (dkey,))
            return post

        run_group(0, C_Z, 1024, simple(zs_tm, "zs_tm", AF.Silu))

        def post_h(c, b, ps, pk, n):
            o = WT[2 + (c + b) % 2][:, 0:512]
            ok = "wt%d" % (2 + (c + b) % 2)
            if b < 2:
                kb.act(o, ps[:, 0:512], AF.Sigmoid, (pk,), (ok,), scale=-1.0)
                kb.tt(o, o, oml_tm[:, l, :], ALU.mult, (ok, "oml_tm"), (ok,))
                kb.dma(key_tm[rows(c), b * 512:(b + 1) * 512], o, (ok,), ("key_tm",))
                g = WT[4 + (c + b) % 2][:, 0:512]
                gk = "wt%d" % (4 + (c + b) % 2)
                kb.ts(g, o, -1.0, 1.0, ALU.mult, ALU.add, (ok,), (gk,))
                kb.act(g, g, AF.Ln, (gk,), (gk,))
                kb.dma(g_tm[rows(c), b * 512:(b + 1) * 512], g, (gk,), ("g_tm",))
            elif b == 2:
                kb.act(o, ps[:, 0:512], AF.Copy, (pk,), (ok,))
                kb.dma(hi_tm[rows(c), :], o, (ok,), ("hi_tm",))
            else:
                kb.act(o, ps[:, 0:512], AF.Silu, (pk,), (ok,))
                kb.dma(hg_tm[rows(c), :], o, (ok,), ("hg_tm",))
        run_group(1, C_HF, 1024, post_h)
        run_group(0, C_HI, 1024, lambda c, b, ps, pk, n: post_h(c, b + 2, ps, pk, n))

        def post_m(c, b, ps, pk, n):
            o = WT[2 + (c + b) % 2][:, 0:512]
            ok = "wt%d" % (2 + (c + b) % 2)
            if b == 0:
                kb.act(o, ps[:, 0:512], AF.Copy, (pk,), (ok,))
                kb.dma(mv_tm[rows(c), :], o, (ok,), ("mv_tm",))
            elif b == 1:
                kb.act(o, ps[:, 0:512], AF.Sigmoid, (pk,), (ok,))
                kb.dma(mo_tm[rows(c), :], o, (ok,), ("mo_tm",))
            else:
                gt = o[:, 0:16]
                kb.tt(gt[:, 0:8], ps[:, 0:8], ppv("ig_bias"), ALU.add, (pk, "pp"), (ok,))
                kb.tt(gt[:, 8:16], ps[:, 8:16], ppv("fg_bias"), ALU.add, (pk, "pp"), (ok,))
                kb.act(gt[:, 8:16], gt[:, 8:16], AF.Exp, (ok,), (ok,), scale=-1.0)
                kb.ts(gt[:, 8:16], gt[:, 8:16], 1.0, None, ALU.add, None, (ok,), (ok,))
                kb.act(gt[:, 8:16], gt[:, 8:16], AF.Ln, (ok,), (ok,))
                kb.ts(gt[:, 8:16], gt[:, 8:16], -1.0, None, ALU.mult, None, (ok,), (ok,))
                kb.dma(gat_tm[rows(c), 64:80], gt, (ok,), ("gat_tm",))
        run_group(0, C_MV, 1040, post_m)

        def post_dt(c, b, ps, pk, n):
            o = WT[2 + c % 2][:, 0:64]
            ok = "wt%d" % (2 + c % 2)
            kb.tt(o[:, 0:32], ps[:, 0:32], ppv("dt_bias"), ALU.add, (pk, "pp"), (ok,))
            kb.act(o[:, 0:32], o[:, 0:32], AF.Exp, (ok,), (ok,))
            kb.ts(o[:, 0:32], o[:, 0:32], 1.0, None, ALU.add, None, (ok,), (ok,))
            kb.act(o[:, 0:32], o[:, 0:32], AF.Ln, (ok,), (ok,))
            kb.tt(o[:, 32:64], o[:, 0:32], ppv("a_log"), ALU.mult, (ok, "pp"), (ok,))
            kb.dma(gat_tm[rows(c), 0:64], o, (ok,), ("gat_tm",))
        run_group(1, C_DT, 32, post_dt)


    gat_sb = kb.sb("gat_sb", [128, 80])
    aall = kb.sb("aall", [128, 40])
    nb = kb.sb("nb", [128, 40])
    wts = kb.sb("wts", [128, 40])
    sc2 = kb.sb("sc2", [128, 32])
    r4 = [kb.sb("r4_%d" % i, [128, 512]) for i in range(2)]
    neg4b = kb.sb("neg4b", [128, 2, 512], BF16)
    hgd_all = kb.sb("hgd_all", [128, 256])
    dec_t = [kb.sb("dec_t%d" % i, [128, 128]) for i in range(2)]
    mt_t = [kb.sb("mt_t%d" % i, [128, 128], BF16) for i in range(2)]
    kb_dall = kb.sb("dall", [128, NCORES, 48])
    dscr = kb.sb("dscr", [128, 1024])
    qhm = [kb.sb("qhm%d" % i, [128, 128], BF16) for i in range(2)]
    for i_ in range(2):
        kb.memset(qhm[i_][:, :], 0.0, ("qhm%d" % i_,))
    for d_ in range(2):
        for j_ in range(4):
            kb.copy(neg4b[:, d_, j_ * 128:(j_ + 1) * 128], consts[:, 4 + d_, :], ("consts",), ("neg4b",))

    def rows(c):
        return slice(c * 128, (c + 1) * 128)

    DIRCOLS = [(0, 16, 0), (16, 32, 1), (32, 36, 0), (36, 40, 1)]

    def chunk_gates(c):
        kb.dma(gat_sb[:], gat_tm[rows(c), :], ("gat_tm",), ("gat_sb",))
        kb.copy(aall[:, 0:32], gat_sb[:, 32:64], ("gat_sb",), ("aall",))
        kb.copy(aall[:, 32:40], gat_sb[:, 72:80], ("gat_sb",), ("aall",))
        ps = PS[2]
        for lo, hi, d in DIRCOLS:
            kb.mm(ps[:, lo:hi], TRIB if d else TRIF, aall[:, lo:hi], True, True, ("aall", "consts"), ("ps2",))
        kb.mm(ps[:, 64:104], ONES, aall[:, 0:40], True, True, ("aall", "consts"), ("ps2",))
        kb.copy(acs_all[:, c, :], ps[:, 0:40], ("ps2",), ("acs_all",), eng="act")
        kb.copy(atot_all[:, c, :], ps[:, 64:104], ("ps2",), ("atot_all",), eng="act")
        kb.ts(nb[:], acs_all[:, c, :], -1.0, None, ALU.mult, None, ("acs_all",), ("nb",))
        kb.tt(nb[:, 32:40], nb[:, 32:40], gat_sb[:, 64:72], ALU.add, ("nb", "gat_sb"), ("nb",))
        kb.tt(wts[:], atot_all[:, c, :], nb[:], ALU.add, ("atot_all", "nb"), ("wts",))
        kb.act(wts[:], wts[:], AF.Exp, ("wts",), ("wts",))
        kb.tt(sc2[:], gat_sb[:, 0:32], wts[:, 0:32], ALU.mult, ("gat_sb", "wts"), ("sc2",))

    def decay_group(gi, h0, d):
        R = r4[gi % 2]
        rk = "r4_%d" % (gi % 2)
        tri = TRIB if d else TRIF
        kb.tt(R[:, :].rearrange("p (h l) -> p h l", h=4),
              aall[:, h0:h0 + 4].unsqueeze(2).to_broadcast([128, 4, 128]),
              tri.unsqueeze(1).to_broadcast([128, 4, 128]), ALU.mult, ("aall", "consts"), (rk,))
        ps = PS[gi % 2]
        pk = "ps%d" % (gi % 2)
        kb.mm(ps[:, :], ONES, R[:, :], True, False, (rk, "consts"), (pk,))
        kb.mm(ps[:, :], identb, neg4b[:, d, :], False, True, ("constb", "neg4b"), (pk,))
        return ps, pk

    def phase_B1(l):
        for c in range(NCH):
            cs = slice(c * 128, (c + 1) * 128)
            chunk_gates(c)
            if "ssd" not in B1_PARTS:
                continue
            bcT = WB[0][:, 0:512].rearrange("p (j t) -> p j t", j=4)
            kb.dmac3(bcT, bc_fm[:, cs].rearrange("(j p) t -> p j t", p=128), 4, ("bc_fm",), ("wb0",))
            xs = WT[0]
            kb.dma(xs[:], xs_tm[cs, :], ("xs_tm",), ("wt0",))
            btm = WB[1][:, 0:256]
            kb.dmac(btm, b_tm[cs, :], ("b_tm",), ("wb1",))
            sc = WT[1]
            for g in range(2):
                kb.mm(PS[3][:, g * 128:(g + 1) * 128], bcT[:, g, :], bcT[:, 2 + g, :], True, True, ("wb0",), ("ps3",))
            kb.copy(sc[:, 0:256], PS[3][:, 0:256], ("ps3",), ("wt1",), eng="act")
            X = [WB[2], WB[3]]
            xs3 = xs[:, :].rearrange("p (h q) -> p h q", h=16)
            for d in range(2):
                kb.tt(X[d][:, :].rearrange("p (h q) -> p h q", h=16), xs3,
                      gat_sb[:, d * 16:(d + 1) * 16].unsqueeze(2).to_broadcast([128, 16, 64]), ALU.mult,
                      ("wt0", "gat_sb"), ("wb%d" % (2 + d),))
            gi = 0
            for d in range(2):
                for q in range(4):
                    ps, pk = decay_group(gi, d * 16 + q * 4, d)
                    for j in range(4):
                        h = q * 4 + j
                        hd = d * 16 + h
                        dec = r4[(gi + 1) % 2]
                        decv = dec_t[j % 2][:, :]
                        mt = mt_t[j % 2][:, :]
                        kb.act(decv, ps[:, j * 128:(j + 1) * 128], AF.Exp, (pk, "nb"), ("dec_t%d" % (j % 2),),
                               bias=nb[:, hd:hd + 1])
                        kb.tt(mt, decv, sc[:, (h // 8) * 128:(h // 8) * 128 + 128], ALU.mult,
                              ("dec_t%d" % (j % 2), "wt1"), ("mt_t%d" % (j % 2),))
                        kb.mm(PS[4 + h // 8][:, (h % 8) * 64:(h % 8) * 64 + 64], mt, X[d][:, h * 64:(h + 1) * 64],
                              d == 0 and h % 8 == 0, d == 1, ("mt_t%d" % (j % 2), "wb%d" % (2 + d)), ("ps%d" % (4 + h // 8),))
                    gi += 1
            yo = WT[2]
            kb.tt(yo[:, :].rearrange("p (h q) -> p h q", h=16), xs3, ppv("dskip").unsqueeze(2).to_broadcast([128, 16, 64]), ALU.mult, ("wt0", "pp"), ("wt2",))
            for hf_ in range(2):
                kb.tt(yo[:, hf_ * 512:(hf_ + 1) * 512], yo[:, hf_ * 512:(hf_ + 1) * 512], PS[4 + hf_][:, :], ALU.add,
                      ("wt2", "ps%d" % (4 + hf_)), ("wt2",))
            kb.dma(yssd_d[cs, :], yo[:], ("wt2",), ("yssd_d",))
            for d in range(2):
                kb.tt(X[d][:, :].rearrange("p (h q) -> p h q", h=16), xs3,
                      sc2[:, d * 16:(d + 1) * 16].unsqueeze(2).to_broadcast([128, 16, 64]), ALU.mult,
                      ("wt0", "sc2"), ("wb%d" % (2 + d),))
                uo = WT[3]
                for g in range(2):
                    kb.mm(PS[6 + g][:, :], btm[:, g * 128:(g + 1) * 128], X[d][:, g * 512:(g + 1) * 512], True, True,
                          ("wb1", "wb%d" % (2 + d)), ("ps%d" % (6 + g),))
                    kb.copy(uo[:, g * 512:(g + 1) * 512], PS[6 + g][:, :], ("ps%d" % (6 + g),), ("wt3",),
                            eng="act" if g else "dve")
                r0 = (d * NCH + c) * 128
                kb.dma(ussd_d[r0:r0 + 128, :], uo[:], ("wt3",), ("ussd_d",))

            if "ml" not in B1_PARTS:
                continue
            qk = WB[0][:, 0:1024].rearrange("p (j t) -> p j t", j=8)
            kb.dmac3(qk, mqk_fm[:, cs].rearrange("(j p) t -> p j t", p=128), 8, ("mqk_fm",), ("wb0",))
            mv1 = WB[1][:, 0:528].rearrange("p (j v) -> p j v", j=4)
            kb.dmac3(mv1[:, :, 0:128], mv_tm[cs, :].rearrange("t (j v) -> t j v", j=4), 4, ("mv_tm",), ("wb1",))
            kb.memset(mv1[:, :, 128:129], 1.0, ("wb1",))
            mk = WT[0][:, 0:512]
            kb.dma(mk, mk_tm[cs, :], ("mk_tm",), ("wt0",))
            msc = WT[1]
            for j in range(4):
                kb.mm(PS[3][:, j * 128:(j + 1) * 128], qk[:, 4 + j, :], qk[:, j, :], True, True, ("wb0",), ("ps3",))
            kb.copy(msc[:, 0:512], PS[3][:, :], ("ps3",), ("wt1",), eng="act")
            for d in range(2):
                ps, pk = decay_group(gi, 32 + d * 4, d)
                gi += 1
                for j in range(4):
                    hd = 32 + d * 4 + j
                    decv = dec_t[j % 2][:, :]
                    mt = mt_t[j % 2][:, :]
                    kb.act(decv, ps[:, j * 128:(j + 1) * 128], AF.Exp, (pk, "nb"), ("dec_t%d" % (j % 2),),
                           bias=nb[:, hd:hd + 1])
                    kb.tt(mt, decv, msc[:, j * 128:(j + 1) * 128], ALU.mult, ("dec_t%d" % (j % 2), "wt1"),
                          ("mt_t%d" % (j % 2),))
                    kb.mm(PS[4 + j // 2][:, (j % 2) * 129:(j % 2) * 129 + 129], mt, mv1[:, j, 0:129], True, True,
                          ("mt_t%d" % (j % 2), "wb1"), ("ps%d" % (4 + j // 2),))
                no = WT[2][:, 0:516]
                for pr in range(2):
                    kb.copy(no[:, pr * 258:(pr + 1) * 258], PS[4 + pr][:, 0:258], ("ps%d" % (4 + pr),), ("wt2",),
                            eng="act" if pr else "dve")
                kb.dma(mln_d[cs, d * 516:(d + 1) * 516], no, ("wt2",), ("mln_d",))
                kw = WB[2][:, 0:512]
                for j in range(4):
                    kb.ts(kw[:, j * 128:(j + 1) * 128], mk[:, j * 128:(j + 1) * 128], wts[:, 32 + d * 4 + j:33 + d * 4 + j],
                          None, ALU.mult, None, ("wt0", "wts"), ("wb2",))
                uo = WT[3][:, 0:516]
                for j in range(4):
                    kb.mm(PS[6 + j // 2][:, (j % 2) * 129:(j % 2) * 129 + 129], kw[:, j * 128:(j + 1) * 128], mv1[:, j, 0:129],
                          True, True, ("wb2", "wb1"), ("ps%d" % (6 + j // 2),))
                for pr in range(2):
                    kb.copy(uo[:, pr * 258:(pr + 1) * 258], PS[6 + pr][:, 0:258], ("ps%d" % (6 + pr),), ("wt3",),
                            eng="act" if pr else "dve")
                r0 = (d * NCH + c) * 128
                kb.dma(uml_d[r0:r0 + 128, :], uo, ("wt3",), ("uml_d",))

            if "hg" not in B1_PARTS:
                continue
            g_sb = WT[0]
            kb.dma(g_sb[:], g_tm[cs, :], ("g_tm",), ("wt0",))
            key_sb = WT[4]
            kb.dma(key_sb[:], key_tm[cs, :], ("key_tm",), ("wt4",))
            hib = WB[1][:, 0:512]
            kb.dmac(hib, hi_tm[cs, :], ("hi_tm",), ("wb1",))
            hqT = WT[5][:, 0:512].rearrange("p (j t) -> p j t", j=4)
            for j_ in range(4):
                kb.dma(hqT[:, j_, :], hq_fm[j_ * 128:(j_ + 1) * 128, cs], ("hq_fm",), ("wt5",))
            keyT = WT[6][:, :].rearrange("p (j t) -> p j t", j=8)
            for j_ in range(8):
                kb.dma(keyT[:, j_, :], key_fm[j_ * 128:(j_ + 1) * 128, cs], ("key_fm",), ("wt6",))
            for d in range(2):
                bt = BTB if d else BTF
                for j in range(4):
                    gcol = g_sb[:, d * 512 + j * 128:d * 512 + (j + 1) * 128]
                    pa = PS[(d * 4 + j) % 2]
                    pak = "ps%d" % ((d * 4 + j) % 2)
                    kb.mm(pa[:, 0:128], gcol, bt, True, True, ("wt0", "consts"), (pak,))
                    kb.mm(pa[:, 128:256], bt, gcol, True, True, ("wt0", "consts"), (pak,))
                    kb.mm(pa[:, 256:384], BONES, gcol, True, True, ("wt0", "consts"), (pak,))
                    gt = WT[7][:, 256:384]
                    kb.copy(gt, pa[:, 0:128], (pak,), ("wt7_g",), eng="act")
                    hidx = (d * 32 + 2 * c) * 4 + j
                    if d == 0:
                        kb.copy(hgd_all[:, hidx:hidx + 1], gt[:, 63:64], ("wt7_g",), ("hgd_all",))
                        kb.copy(hgd_all[:, hidx + 4:hidx + 5], gt[:, 127:128], ("wt7_g",), ("hgd_all",))
                    else:
                        kb.copy(hgd_all[:, hidx:hidx + 1], gt[:, 0:1], ("wt7_g",), ("hgd_all",))
                        kb.copy(hgd_all[:, hidx + 4:hidx + 5], gt[:, 64:65], ("wt7_g",), ("hgd_all",))
                    nref = WT[7][:, 384:386]
                    for blk in range(2):
                        kb.ts(nref[:, blk:blk + 1], gt[:, blk * 64 + 31:blk * 64 + 32], -1.0, None, ALU.mult, None, ("wt7_g",), ("wt7_e",))
                    ep = WT[7][:, 512:640]
                    en = WT[7][:, 640:768]
                    for blk in range(2):
                        bs = slice(blk * 64, (blk + 1) * 64)
                        kb.act(ep[:, bs], gt[:, bs], AF.Exp, ("wt7_g", "wt7_e"), ("wt7_ep",), bias=nref[:, blk:blk + 1])
                        kb.act(en[:, bs], gt[:, bs], AF.Exp, ("wt7_g", "wt7_e"), ("wt7_en",), bias=gt[:, blk * 64 + 31:blk * 64 + 32], scale=-1.0)
                    qt = WB[2][:, 0:128]
                    kt = WB[2][:, 128:256]
                    kb.tt(qt, hqT[:, j, :], ep, ALU.mult, ("wt5", "wt7_ep"), ("wb2q",))
                    kb.tt(kt, keyT[:, d * 4 + j, :], en, ALU.mult, ("wt6", "wt7_en"), ("wb2k",))
                    kb.mm(PS[2][:, 0:128], kt, qt, True, True, ("wb2q", "wb2k"), ("ps2",))
                    att = WB[3][:, 0:128]
                    kb.tt(att, PS[2][:, 0:128], bt, ALU.mult, ("ps2", "consts"), ("wb3",))
                    kb.mm(PS[4][:, j * 128:(j + 1) * 128], att, hib[:, j * 128:(j + 1) * 128], d == 0 and j == 0, d == 1,
                          ("wb3", "wb1"), ("ps4",))
                    dd = WT[7][:, 768:896]
                    gtm_c = WT[7][:, 896:1024]
                    kb.copy(gtm_c, pa[:, 128:256], (pak,), ("wt7_d",), eng="act")
                    kb.tt(dd, pa[:, 256:384], gtm_c, ALU.subtract, (pak, "wt7_d"), ("wt7_d",))
                    kb.act(dd, dd, AF.Exp, ("wt7_d",), ("wt7_d",))
                    kend = WB[4][:, 0:128]
                    kb.tt(kend, key_sb[:, d * 512 + j * 128:d * 512 + (j + 1) * 128], dd, ALU.mult, ("wt4", "wt7_d"),
                          ("wb4",))
                    for blk in range(2):
                        kmask = WB[4][:, 128 + blk * 128:256 + blk * 128]
                        kb.ts(kmask, kend, BONES[:, blk * 127:blk * 127 + 1], None, ALU.mult, None, ("wb4", "consts"), ("wb4",))
                        kb.mm(PS[5][:, ((j % 2) * 2 + blk) * 128:((j % 2) * 2 + blk) * 128 + 128], kmask, hib[:, j * 128:(j + 1) * 128],
                              True, True, ("wb4", "wb1"), ("ps5",))
                    if j % 2 == 1:
                        uo = WT[3][:, 0:512]
                        kb.copy(uo, PS[5][:, :], ("ps5",), ("wt3",), eng="act")
                        for jj in range(2):
                            for blk in range(2):
                                r0 = (d * 2 * NCH + 2 * c + blk) * 128
                                jh = j - 1 + jj
                                kb.dma(uhg_d[r0:r0 + 128, jh * 128:(jh + 1) * 128],
                                       uo[:, (jj * 2 + blk) * 128:(jj * 2 + blk) * 128 + 128], ("wt3",), ("uhg_d",))
            ho = WT[2][:, 0:512]
            kb.copy(ho, PS[4][:, :], ("ps4",), ("wt2",))
            kb.dma(hgo_d[cs, :], ho, ("wt2",), ("hgo_d",))


    cmask_in = inp("cmask", [128, 16])
    cmask = kb.sb("cmask", [128, 16])
    kb.dma(cmask[:], cmask_in, (), ("cmask",))
    pcum = kb.sb("pcum", [128, 2, NCH, 40])
    hgp = kb.sb("hgp", [128, 256])
    dsm = kb.sb("dsm", [128, 48])
    dtmp = kb.sb("dtmp", [128, 48])

    MIX = [("ssd", ussd_d, sssd_d, 1024, NCH, 0, 1024),
           ("ml", uml_d, sml_d, 516, NCH, 2048, 2564),
           ("hg", uhg_d, shg_d, 512, 2 * NCH, 3080, 3592)]

    def decay_mul(name, st, stk, d, step, src_tile, src_keys, logit=True):
        if name == "ssd":
            ld = src_tile("ssd", d, step)
            kb.act(dtmp[:, 0:16], ld, AF.Exp, src_keys, ("dtmp",))
            kb.tt(st[:, 0:1024].rearrange("p (h q) -> p h q", h=16), st[:, 0:1024].rearrange("p (h q) -> p h q", h=16),
                  dtmp[:, 0:16].unsqueeze(2).to_broadcast([128, 16, 64]), ALU.mult, (stk, "dtmp"), (stk,))
        elif name == "ml":
            ld = src_tile("ml", d, step)
            kb.act(dtmp[:, 16:20], ld, AF.Exp, src_keys, ("dtmp",))
            kb.tt(st[:, 0:516].rearrange("p (h q) -> p h q", h=4), st[:, 0:516].rearrange("p (h q) -> p h q", h=4),
                  dtmp[:, 16:20].unsqueeze(2).to_broadcast([128, 4, 129]), ALU.mult, (stk, "dtmp"), (stk,))
        else:
            ld = src_tile("hg", d, step)
            kb.act(dtmp[:, 20:24], ld, AF.Exp, src_keys, ("dtmp",))
            kb.tt(st[:, 0:512].rearrange("p (h q) -> p h q", h=4), st[:, 0:512].rearrange("p (h q) -> p h q", h=4),
                  dtmp[:, 20:24].unsqueeze(2).to_broadcast([128, 4, 128]), ALU.mult, (stk, "dtmp"), (stk,))

    def chunk_ld(name, d, step):
        if name == "ssd":
            return atot_all[:, step, d * 16:(d + 1) * 16]
        if name == "ml":
            return atot_all[:, step, 32 + d * 4:36 + d * 4]
        return hgd_all[:, (d * 32 + step) * 4:(d * 32 + step) * 4 + 4]

    def phase_S(l):
        for d in range(2):
            order = list(range(NCH)) if d == 0 else list(range(NCH - 1, -1, -1))
            kb.memset(pcum[:, d, order[0], :], 0.0, ("pcum",))
            for i in range(1, NCH):
                kb.tt(pcum[:, d, order[i], :], pcum[:, d, order[i - 1], :], atot_all[:, order[i - 1], :], ALU.add,
                      ("pcum", "atot_all"), ("pcum",))
            order = list(range(32)) if d == 0 else list(range(31, -1, -1))
            o0 = (d * 32 + order[0]) * 4
            kb.memset(hgp[:, o0:o0 + 4], 0.0, ("hgp",))
            for i in range(1, 32):
                oc, op_ = (d * 32 + order[i]) * 4, (d * 32 + order[i - 1]) * 4
                kb.tt(hgp[:, oc:oc + 4], hgp[:, op_:op_ + 4], hgd_all[:, op_:op_ + 4], ALU.add, ("hgp", "hgd_all"), ("hgp",))
        summ = WT[7]
        for mi, (name, U_d, S_d, width, nst, off_f, off_b) in enumerate(MIX):
            for d in range(2):
                st = WT[mi * 2 + d]
                stk = "wt%d" % (mi * 2 + d)
                kb.memset(st[:, 0:width], 0.0, (stk,))
                order = list(range(nst)) if d == 0 else list(range(nst - 1, -1, -1))
                for step in order:
                    r0 = (d * nst + step) * 128
                    kb.dma(S_d[r0:r0 + 128, :], st[:, 0:width], (stk,), ("S_" + name,))
                    ut = WT[6 + (step % 2)]
                    uk = "wt%d" % (6 + step % 2)
                    kb.dma(ut[:, 0:width], U_d[r0:r0 + 128, :], ("u" + name + "_d",), (uk,))
                    decay_mul(name, st, stk, d, step, chunk_ld, ("atot_all", "hgd_all"))
                    kb.tt(st[:, 0:width], st[:, 0:width], ut[:, 0:width], ALU.add, (stk, uk), (stk,))
                off = off_b if d else off_f
                kb.dma(sum_d[:, off:off + width], st[:, 0:width], (stk,), ("sum_d",))
        kb.tt(dsm[:, 0:40], pcum[:, 0, NCH - 1, :], atot_all[:, NCH - 1, :], ALU.add, ("pcum", "atot_all"), ("dsm",))
        for d in range(2):
            last = 31 if d == 0 else 0
            oc = (d * 32 + last) * 4
            kb.tt(dsm[:, 40 + d * 4:44 + d * 4], hgp[:, oc:oc + 4], hgd_all[:, oc:oc + 4], ALU.add, ("hgp", "hgd_all"), ("dsm",))
        kb.dma(sum_d[:, 4104:4152], dsm[:, 0:48], ("dsm",), ("sum_d",))
        kb.allgather(sum_all[:, :], sum_d[:, :], ("sum_d",), ("sum_all",))
        dall = kb_dall
        for j in range(NCORES):
            kb.dma(dall[:, j, :], sum_all[j * 128:(j + 1) * 128, 4104:4152], ("sum_all",), ("dall",))
        for j in range(NCORES):
            for d in range(2):
                kb.ts(dall[:, j, d * 16:(d + 1) * 16], dall[:, j, d * 16:(d + 1) * 16], cmask[:, d * 8 + j:d * 8 + j + 1], None,
                      ALU.mult, None, ("dall", "cmask"), ("dall",))
                kb.ts(dall[:, j, 32 + d * 4:36 + d * 4], dall[:, j, 32 + d * 4:36 + d * 4], cmask[:, d * 8 + j:d * 8 + j + 1], None,
                      ALU.mult, None, ("dall", "cmask"), ("dall",))
                kb.ts(dall[:, j, 40 + d * 4:44 + d * 4], dall[:, j, 40 + d * 4:44 + d * 4], cmask[:, d * 8 + j:d * 8 + j + 1], None,
                      ALU.mult, None, ("dall", "cmask"), ("dall",))

        def core_ld(name, d, j):
            if name == "ssd":
                return dall[:, j, d * 16:(d + 1) * 16]
            if name == "ml":
                return dall[:, j, 32 + d * 4:36 + d * 4]
            return dall[:, j, 40 + d * 4:44 + d * 4]

        for mi, (name, U_d, S_d, width, nst, off_f, off_b) in enumerate(MIX):
            for d in range(2):
                st = WT[mi * 2 + d]
                stk = "wt%d" % (mi * 2 + d)
                kb.memset(st[:, 0:width], 0.0, (stk,))
                off = off_b if d else off_f
                order = list(range(NCORES)) if d == 0 else list(range(NCORES - 1, -1, -1))
                for j in order:
                    ft = WT[6 + (j % 2)]
                    fk = "wt%d" % (6 + j % 2)
                    kb.dma(ft[:, 0:width], sum_all[j * 128:(j + 1) * 128, off:off + width], ("sum_all",), (fk,))
                    decay_mul(name, st, stk, d, j, core_ld, ("dall",))
                    kb.stt(st[:, 0:width], ft[:, 0:width], cmask[:, d * 8 + j:d * 8 + j + 1], st[:, 0:width], ALU.mult, ALU.add,
                           (fk, "cmask", stk), (stk,))
                for step in range(nst):
                    r0 = (d * nst + step) * 128
                    s0 = WT[6 + (step % 2)]
                    sk = "wt%d" % (6 + step % 2)
                    kb.dma(s0[:, 0:width], S_d[r0:r0 + 128, :], ("S_" + name,), (sk,))
                    if name == "ssd":
                        kb.act(dtmp[:, 0:16], pcum[:, d, step, d * 16:(d + 1) * 16], AF.Exp, ("pcum",), ("dtmp",))
                        hh, qq, dv = 16, 64, dtmp[:, 0:16]
                    elif name == "ml":
                        kb.act(dtmp[:, 16:20], pcum[:, d, step, 32 + d * 4:36 + d * 4], AF.Exp, ("pcum",), ("dtmp",))
                        hh, qq, dv = 4, 129, dtmp[:, 16:20]
                    else:
                        oc = (d * 32 + step) * 4
                        kb.act(dtmp[:, 20:24], hgp[:, oc:oc + 4], AF.Exp, ("hgp",), ("dtmp",))
                        hh, qq, dv = 4, 128, dtmp[:, 20:24]
                    tmp = WT[7 if name != "hg" else 7]
                    tv = r4[0] if False else None
                    kb.tt(dscr[:, 0:width].rearrange("p (h q) -> p h q", h=hh), st[:, 0:width].rearrange("p (h q) -> p h q", h=hh),
                          dv.unsqueeze(2).to_broadcast([128, hh, qq]), ALU.mult, (stk, "dtmp"), ("dscr",))
                    kb.tt(s0[:, 0:width], s0[:, 0:width], dscr[:, 0:width], ALU.add, (sk, "dscr"), (sk,))
                    kb.dma(S_d[r0:r0 + 128, :], s0[:, 0:width], (sk,), ("S_" + name,))


    ALPHA = (2.0 * DEPTH) ** 0.25
    ecum = kb.sb("ecum", [128, 40])
    gsm = kb.sb("gsm", [128, 32])
    router_sb = kb.sb("router_sb", [128, 8, 8])

    def group_rstd(x3, G, n, eps, xk):
        sq = dscr[:, 0:G * n].rearrange("p (g n) -> p g n", g=G)
        kb.tt(sq, x3, x3, ALU.mult, (xk,), ("dscr",))
        kb.red(gsm[:, 0:G], sq, ALU.add, ("dscr",), ("gsm",))
        kb.ts(gsm[:, 0:G], gsm[:, 0:G], 1.0 / n, eps, ALU.mult, ALU.add, ("gsm",), ("gsm",))
        kb.act(gsm[:, 0:G], gsm[:, 0:G], AF.Ln, ("gsm",), ("gsm",))
        kb.act(gsm[:, 0:G], gsm[:, 0:G], AF.Exp, ("gsm",), ("gsm",), scale=-0.5)
        kb.tt(x3, x3, gsm[:, 0:G].unsqueeze(2).to_broadcast([128, G, n]), ALU.mult, (xk, "gsm"), (xk,))

    def emit_hT2(c, hsrc, Rk, f32dst=None, f32k=None):
        for half in range(2):
            ps = PS[6 + half]
            pk = "ps%d" % (6 + half)
            for k4 in range(4):
                k = half * 4 + k4
                kb.tr(ps[:, k4 * 128:(k4 + 1) * 128], hsrc[:, k * 128:(k + 1) * 128], IDENT, tuple(Rk) + ("consts",), (pk,))
            kb.copy(hT[:, half * 4:half * 4 + 4, 2 + c * 128:2 + (c + 1) * 128],
                    ps[:, :].rearrange("p (k t) -> p k t", k=4), (pk,), ("hT",), eng="act" if half else "dve")
            if f32dst is not None:
                kb.copy(f32dst[:, half * 512:(half + 1) * 512], ps[:, :], (pk,), (f32k,), eng="act" if half else "dve")

    def phase_B2C(l):
        moe = (l % 2 == 1)
        wo2 = [WSET[i_][:, 0:8192].rearrange("p (k n) -> p k n", k=8) for i_ in range(2)]
        for i_ in range(2):
            kb.dmac3(wo2[i_], wout_g[l][i_ * 1024:(i_ + 1) * 1024, :].rearrange("(k p) c -> p k c", p=128), 8, (("wout", l),), ("wset%d" % i_,))
        if moe:
            kb.dma(router_sb[:, :, :].rearrange("p k e -> p (k e)"), router_in[l // 2], (), ("router_sb",))
        for c in range(NCH):
            cs = slice(c * 128, (c + 1) * 128)
            kb.act(ecum[:], acs_all[:, c, :], AF.Exp, ("acs_all",), ("ecum",))
            mixa, mixb = WT[2], WT[3]
            CT = WB[0][:, 0:256].rearrange("p (j t) -> p j t", j=2)
            kb.dmac3(CT, bc_fm[256:512, cs].rearrange("(j p) t -> p j t", p=128), 2, ("bc_fm",), ("wb0a",))
            y = mixa
            kb.dma(y[:], yssd_d[cs, :], ("yssd_d",), ("wt2",))
            zs = WT[1]
            kb.dma(zs[:], zs_tm[cs, :], ("zs_tm",), ("wt1",))
            for d in range(2):
                S = WB[2 + d]
                r0 = (d * NCH + c) * 128
                kb.dmac(S[:], sssd_d[r0:r0 + 128, :], ("S_ssd",), ("wb%d" % (2 + d),))
                for g in range(2):
                    kb.mm(PS[g][:, :], CT[:, g, :], S[:, g * 512:(g + 1) * 512], True, True, ("wb0a", "wb%d" % (2 + d)), ("ps%d" % g,))
                    t3 = dscr[:, 0:512].rearrange("p (h q) -> p h q", h=8)
                    kb.tt(t3, PS[g][:, :].rearrange("p (h q) -> p h q", h=8),
                          ecum[:, d * 16 + g * 8:d * 16 + g * 8 + 8].unsqueeze(2).to_broadcast([128, 8, 64]), ALU.mult,
                          ("ps%d" % g, "ecum"), ("dscr",))
                    kb.tt(y[:, g * 512:(g + 1) * 512], y[:, g * 512:(g + 1) * 512], dscr[:, 0:512], ALU.add, ("wt2", "dscr"), ("wt2",))
            kb.tt(y[:], y[:], zs[:], ALU.mult, ("wt2", "wt1"), ("wt2",))
            group_rstd(y[:, :].rearrange("p (g n) -> p g n", g=2), 2, 512, 1e-6, "wt2")
            kb.tt(y[:], y[:], ppv("ssd_nw"), ALU.mult, ("wt2", "pp"), ("wt2",))
            g_sb = WT[4]
            kb.dma(g_sb[:], g_tm[cs, :], ("g_tm",), ("wt4",))
            hqT = WT[5][:, 0:512].rearrange("p (j t) -> p j t", j=4)
            for j_ in range(4):
                kb.dma(hqT[:, j_, :], hq_fm[j_ * 128:(j_ + 1) * 128, cs], ("hq_fm",), ("wt5",))
            o = mixb[:, 0:512]
            kb.dma(o, hgo_d[cs, :], ("hgo_d",), ("wt3a",))
            for d in range(2):
                Sh = WB[4 + d]
                for blk in range(2):
                    r0 = (d * 2 * NCH + 2 * c + blk) * 128
                    kb.dmac(Sh[:, blk * 512:(blk + 1) * 512], shg_d[r0:r0 + 128, :], ("S_hg",), ("wb%d" % (4 + d),))
            for d in range(2):
                bt = BTB if d else BTF
                for j in range(4):
                    gcol = g_sb[:, d * 512 + j * 128:d * 512 + (j + 1) * 128]
                    kb.mm(PS[2][:, 0:128], gcol, bt, True, True, ("wt4", "consts"), ("ps2",))
                    eg_ = WT[7][:, 0:128]
                    kb.act(eg_, PS[2][:, 0:128], AF.Exp, ("ps2",), ("wt7_0",))
                    for blk in range(2):
                        bs = slice(blk * 64, (blk + 1) * 64)
                        kb.tt(qhm[blk][:, bs], hqT[:, j, bs], eg_[:, bs], ALU.mult, ("wt5", "wt7_0"), ("qhm%d" % blk,))
                        kb.mm(PS[3][:, j * 128:(j + 1) * 128], qhm[blk][:, :],
                              WB[4 + d][:, blk * 512 + j * 128:blk * 512 + (j + 1) * 128], d == 0 and j == 0 and blk == 0, d == 1 and blk == 1,
                              ("qhm%d" % blk, "wb%d" % (4 + d)), ("ps3",))
            kb.tt(o, o, PS[3][:, :], ALU.add, ("wt3a", "ps3"), ("wt3a",))
            group_rstd(o.rearrange("p (g n) -> p g n", g=4), 4, 128, 1e-6, "wt3a")
            kb.tt(o, o, ppv("hg_nw"), ALU.mult, ("wt3a", "pp"), ("wt3a",))
            hgt = WT[5][:, 512:1024]
            kb.dma(hgt, hg_tm[cs, :], ("hg_tm",), ("wt5b",))
            kb.tt(o, o, hgt, ALU.mult, ("wt3a", "wt5b"), ("wt3a",))
            qT = WB[0][:, 256:768].rearrange("p (j t) -> p j t", j=4)
            kb.dmac3(qT, mqk_fm[0:512, cs].rearrange("(j p) t -> p j t", p=128), 4, ("mqk_fm",), ("wb0b",))
            hm = mixb[:, 512:1024]
            for d in range(2):
                Sm3 = WB[2 + d][:, 0:528].rearrange("p (j v) -> p j v", j=4)
                r0 = (d * NCH + c) * 128
                kb.dmac3(Sm3[:, :, 0:129], sml_d[r0:r0 + 128, :].rearrange("k (j v) -> k j v", j=4), 4, ("S_ml",), ("wb%d" % (2 + d),))
                ni = WT[6][:, 0:516]
                kb.dma(ni, mln_d[cs, d * 516:(d + 1) * 516], ("mln_d",), ("wt6",))
                for j in range(4):
                    pj = PS[4 + j // 2][:, (j % 2) * 129:(j % 2) * 129 + 129]
                    pjk = "ps%d" % (4 + j // 2)
                    kb.mm(pj, qT[:, j, :], Sm3[:, j, 0:129], True, True, ("wb0b", "wb%d" % (2 + d)), (pjk,))
                    nj = ni[:, j * 129:(j + 1) * 129]
                    hd = 32 + d * 4 + j
                    kb.stt(nj, pj, ecum[:, hd:hd + 1], nj, ALU.mult, ALU.add, (pjk, "ecum", "wt6"), ("wt6",))
                    den = gsm[:, 16 + j:17 + j]
                    nx_ = gsm[:, 20 + j:21 + j]
                    kb.ts(nx_, nj[:, 128:129], -1.0, None, ALU.mult, None, ("wt6",), ("gsm",))
                    kb.tt(den, nj[:, 128:129], nx_, ALU.max, ("wt6", "gsm"), ("gsm",))
                    kb.ts(den, den, 1.0, None, ALU.max, None, ("gsm",), ("gsm",))
                    kb.recip(den, den, ("gsm",), ("gsm",))
                    if d == 0:
                        kb.ts(hm[:, j * 128:(j + 1) * 128], nj[:, 0:128], den, None, ALU.mult, None, ("wt6", "gsm"), ("wt3b",))
                    else:
                        kb.stt(hm[:, j * 128:(j + 1) * 128], nj[:, 0:128], den, hm[:, j * 128:(j + 1) * 128], ALU.mult, ALU.add,
                               ("wt6", "gsm", "wt3b"), ("wt3b",))
            hm3 = hm.rearrange("p (g n) -> p g n", g=4)
            kb.red(gsm[:, 8:12], hm3, ALU.add, ("wt3b",), ("gsm",))
            kb.ts(gsm[:, 8:12], gsm[:, 8:12], -1.0 / 128, None, ALU.mult, None, ("gsm",), ("gsm",))
            kb.tt(hm3, hm3, gsm[:, 8:12].unsqueeze(2).to_broadcast([128, 4, 128]), ALU.add, ("wt3b", "gsm"), ("wt3b",))
            group_rstd(hm3, 4, 128, 1e-5, "wt3b")
            kb.tt(hm, hm, ppv("ml_nw"), ALU.mult, ("wt3b", "pp"), ("wt3b",))
            mot = WT[6][:, 516:1028] if False else WT[1][:, 0:512]
            kb.dma(mot, mo_tm[cs, :], ("mo_tm",), ("wt1",))
            kb.tt(hm, hm, mot, ALU.mult, ("wt3b", "wt1"), ("wt3b",))
            if DEBUG_DUMP:
                kb.dma(mix_d[cs, 0:1024], mixa[:], ("wt2",), ("mix_d",))
                kb.dma(mix_d[cs, 1024:2048], mixb[:], ("wt3a", "wt3b"), ("mix_d",))
            mixT = [WB[4], WB[5]]
            for q in range(4):
                src = mixa if q < 2 else mixb
                srck = ("wt2",) if q < 2 else ("wt3a", "wt3b")
                ps = PS[q % 2]
                pk = "ps%d" % (q % 2)
                for m4 in range(4):
                    col = (q % 2) * 512 + m4 * 128
                    kb.tr(ps[:, m4 * 128:(m4 + 1) * 128], src[:, col:col + 128], IDENT, srck + ("consts",), (pk,))
                kb.copy(mixT[q // 2][:, (q % 2) * 512:(q % 2) * 512 + 512], ps[:, :], (pk,), ("wb%d" % (4 + q // 2),),
                        eng="act" if q % 2 else "dve")
            for dh in range(2):
                for m in range(16):
                    kb.mm(PS[2 + dh][:, :], mixT[m // 8][:, (m % 8) * 128:(m % 8) * 128 + 128], wo2[m // 8][:, m % 8, dh * 512:(dh + 1) * 512],
                          m == 0, m == 15, ("wb4", "wb5", "wset0", "wset1"), ("ps%d" % (2 + dh),))
            hres = WT[7]
            kb.dma(hres[:], h_d[cs, :], ("h_d",), ("wt7_0", "wt7_1", "wt7_g", "wt7_e", "wt7_ep", "wt7_en", "wt7_d"))
            pre = WT[0]
            for dh in range(2):
                kb.stt(pre[:, dh * 512:(dh + 1) * 512], hres[:, dh * 512:(dh + 1) * 512], ALPHA, PS[2 + dh][:, :], ALU.mult, ALU.add,
                       ("wt7_0", "ps%d" % (2 + dh)), ("wt0",))
            h1 = WT[4]
            layernorm(h1[:], pre[:], ppv("ln1_g"), ppv("ln1_b"), ("wt0", "pp"), ("wt4",))
            kb.dma(h_d[cs, :], h1[:], ("wt4",), ("h_d",))
            if DEBUG_DUMP:
                kb.dma(h1_d[cs, :], h1[:], ("wt4",), ("h1_d",))
            if moe:
                hf32 = WT[6]
                emit_hT2(c, h1, ("wt4",), hf32, "wt6")
                for k in range(8):
                    kb.mm(PS[5][:, k * 8:(k + 1) * 8], hf32[:, k * 128:(k + 1) * 128], router_sb[:, k, :], True, True, ("wt6", "router_sb"), ("ps5",))
                lg = gsm[:, 0:8]
                lgp = small[:, 40:104]
                kb.copy(lgp, PS[5][:, 0:64], ("ps5",), ("small",))
                kb.tt(lg, lgp[:, 0:8], lgp[:, 8:16], ALU.add, ("small",), ("gsm",))
                for k in range(2, 8):
                    kb.tt(lg, lg, lgp[:, k * 8:(k + 1) * 8], ALU.add, ("small", "gsm"), ("gsm",))
                m1 = gsm[:, 8:9]
                def max8(dst, src):
                    t4 = small[:, 32:36]
                    kb.tt(t4, src[:, 0:4], src[:, 4:8], ALU.max, ("gsm",), ("small",))
                    kb.tt(t4[:, 0:2], t4[:, 0:2], t4[:, 2:4], ALU.max, ("small",), ("small",))
                    kb.tt(dst, t4[:, 0:1], t4[:, 1:2], ALU.max, ("small",), ("gsm",))
                max8(m1, lg)
                eq = gsm[:, 16:24]
                kb.act(eq, lg, AF.Sign, ("gsm",), ("gsm",), bias=m1, scale=-1.0)
                kb.ts(eq, eq, -1.0, 1.0, ALU.mult, ALU.add, ("gsm",), ("gsm",))
                kb.stt(eq, eq, -1e30, lg, ALU.mult, ALU.add, ("gsm",), ("gsm",))
                m2 = gsm[:, 9:10]
                max8(m2, eq)
                sel_ = gsm[:, 24:32]
                kb.act(sel_, lg, AF.Sign, ("gsm",), ("gsm",), bias=m2, scale=-1.0)
                kb.ts(sel_, sel_, 0.0, None, ALU.max, None, ("gsm",), ("gsm",))
                kb.ts(sel_, sel_, -1.0, 1.0, ALU.mult, ALU.add, ("gsm",), ("gsm",))
                nm1 = gsm[:, 10:11]
                kb.ts(nm1, m1, -1.0, None, ALU.mult, None, ("gsm",), ("gsm",))
                kb.act(eq, lg, AF.Exp, ("gsm",), ("gsm",), bias=nm1)
                kb.tt(eq, eq, sel_, ALU.mult, ("gsm",), ("gsm",))
                ssum = gsm[:, 11:12]
                kb.red(ssum, eq, ALU.add, ("gsm",), ("gsm",))
                kb.recip(ssum, ssum, ("gsm",), ("gsm",))
                kb.ts(gates_all[:, c, :], eq, ssum, None, ALU.mult, None, ("gsm",), ("gates_all",))
            else:
                emit_hT2(c, h1, ("wt4",))

    def phase_F(l):
        moe = (l % 2 == 1)
        i = l // 2
        jobs = []
        if moe:
            for e in range(8):
                for f0 in range(0, 28, 5):
                    jobs.append((m1_g[i], m3_g[i], m2_g[i], e * D, e * 3584, f0, min(5, 28 - f0), e, ("m1", i), ("m3", i), ("m2", i)))
        else:
            for f0 in range(0, 22, 5):
                jobs.append((f1_g[i], f3_g[i], f2_g[i], 0, 0, f0, min(5, 22 - f0), None, ("f1", i), ("f3", i), ("f2", i)))
        for ji, (w1, w3, w2, r13, r2, f0, nf, e, k1, k3, k2) in enumerate(jobs):
            ai = ji % 2
            ar = WSET[ai]
            ak = "wset%d" % ai
            w1s = ar[:, 0:8 * nf * 128].rearrange("p (k n) -> p k n", k=8)
            w3s = ar[:, 5120:5120 + 8 * nf * 128].rearrange("p (k n) -> p k n", k=8)
            w2s = ar[:, 10240:10240 + nf * 1024].rearrange("p (j n) -> p j n", j=nf)
            if e is None:
                kb.dmac3(w1s, w1[r13:r13 + D, f0 * 128:(f0 + nf) * 128].rearrange("(k p) f -> p k f", p=128), 8, (k1,), (ak,))
                kb.dmac3(w3s, w3[r13:r13 + D, f0 * 128:(f0 + nf) * 128].rearrange("(k p) f -> p k f", p=128), 8, (k3,), (ak,))
                kb.dmac3(w2s, w2[r2 + f0 * 128:r2 + (f0 + nf) * 128, :].rearrange("(j p) d -> p j d", p=128), nf, (k2,), (ak,))
            else:
                for k in range(8):
                    kb.dmac(w1s[:, k, :], w1[k][e * 128:(e + 1) * 128, f0 * 128:(f0 + nf) * 128], (k1,), (ak,))
                    kb.dmac(w3s[:, k, :], w3[k][e * 128:(e + 1) * 128, f0 * 128:(f0 + nf) * 128], (k3,), (ak,))
                for j in range(nf):
                    frow = (f0 + j) * 128
                    kb.dmac(w2s[:, j, :], w2[frow // 512][e * 512 + frow % 512:e * 512 + frow % 512 + 128, :], (k2,), (ak,))
            for tb in range(8):
                tcols = slice(2 + tb * 256, 2 + (tb + 1) * 256)
                for j in range(nf):
                    for k in range(8):
                        kb.mm(PS[0][:, 0:256], w1s[:, k, j * 128:(j + 1) * 128], hT[:, k, tcols], k == 0, k == 7, (ak, "hT"), ("ps0",))
                    for k in range(8):
                        kb.mm(PS[1][:, 0:256], w3s[:, k, j * 128:(j + 1) * 128], hT[:, k, tcols], k == 0, k == 7, (ak, "hT"), ("ps1",))
                    a1 = WT[j % 2][:, 0:256]
                    a1k = "wt%d" % (j % 2)
                    kb.act(a1, PS[0][:, 0:256], AF.Silu, ("ps0",), (a1k,))
                    ab = WB[j % 2][:, 0:256]
                    abk = "wb%d" % (j % 2)
                    kb.tt(ab, a1, PS[1][:, 0:256], ALU.mult, (a1k, "ps1"), (abk,))
                    for th in range(2):
                        for dh in range(2):
                            kb.mm(PS[2 + th * 2 + dh][:, :], ab[:, th * 128:(th + 1) * 128], w2s[:, j, dh * 512:(dh + 1) * 512],
                                  j == 0, j == nf - 1, (abk, ak), ("ps%d" % (2 + th * 2 + dh),))
                for th in range(2):
                    c = tb * 2 + th
                    stg = WT[2 + th]
                    sk = "wt%d" % (2 + th)
                    for dh in range(2):
                        pk = "ps%d" % (2 + th * 2 + dh)
                        if e is None:
                            kb.copy(stg[:, dh * 512:(dh + 1) * 512], PS[2 + th * 2 + dh][:, :], (pk,), (sk,), eng="act" if dh else "dve")
                        else:
                            kb.ts(stg[:, dh * 512:(dh + 1) * 512], PS[2 + th * 2 + dh][:, :], gates_all[:, c, e:e + 1], None, ALU.mult, None,
                                  (pk, "gates_all"), (sk,))
                    fk = ("facc", c)
                    if ji == 0:
                        kb.dma(facc_d[c * 128:(c + 1) * 128, :], stg[:], (sk,), (fk,))
                    else:
                        kb.s.op("dpool", lambda en, o_=facc_d[c * 128:(c + 1) * 128, :], i_=stg[:]: en.dma_start(out=o_, in_=i_, accum_op=ALU.add),
                                (sk, fk), (fk,))

    def phase_C2(l):
        for c in range(NCH):
            cs = slice(c * 128, (c + 1) * 128)
            f = WT[4]
            kb.dma(f[:], facc_d[cs, :], (("facc", c),), ("wt4",))
            h1 = WT[5]
            kb.dma(h1[:], h_d[cs, :], ("h_d",), ("wt5", "wt5b"))
            pre = WT[6]
            kb.stt(pre[:], h1[:], ALPHA, f[:], ALU.mult, ALU.add, ("wt4", "wt5"), ("wt6",))
            hn = WT[7]
            allk7 = ("wt7_0", "wt7_1", "wt7_g", "wt7_e", "wt7_ep", "wt7_en", "wt7_d")
            layernorm(hn[:], pre[:], ppv("ln2_g"), ppv("ln2_b"), ("wt6", "pp"), allk7)
            kb.dma(h_d[cs, :], hn[:], ("wt7_0",), ("h_d",))
            if l < DEPTH - 1:
                emit_hT2(c, hn, ("wt7_0",))
                if c == 0:
                    kb.dma(edge_d[0:2, :], hn[0:2, :], ("wt7_0",), ("edge_d",))
                if c == NCH - 1:
                    kb.dma(edge_d[2:4, :], hn[126:128, :], ("wt7_0",), ("edge_d",))

    NLAYERS = L_RUN
    for l in range(NLAYERS):
        if l + 1 < NLAYERS:
            gather_layer(l + 1)
        load_layer_params(l)
        halo()
        stop = False
        for pname, pfn in (("A1", phase_A1), ("A2", phase_A2), ("B1", phase_B1), ("S", phase_S), ("B2C", phase_B2C),
                           ("F", phase_F), ("C2", phase_C2)):
            pfn(l)
            if STOP_AFTER == (l, pname):
                stop = True
                break
        if stop:
            break

    return nc, kb, locals()


def finish(nc, kb, L, src_key="h_d"):
    y_out, h_d = L["y_out"], L["h_d"]
    last = kb.dma(y_out[:, :], h_d[:, :], (src_key,), ("y",))
    kb.s.emit([last])
    return nc


def make_inputs(inputs, n_layers=DEPTH):
    g = lambda n: np.ascontiguousarray(np.asarray(inputs[n], dtype=np.float32))
    x = g("x").reshape(NCORES, T, D)
    consts = make_consts()
    lnin = np.stack([np.broadcast_to(g("ln_in_g"), (128, D)), np.broadcast_to(g("ln_in_b"), (128, D))], axis=1)
    pp = np.zeros((DEPTH, 128, NPP), np.float32)

    def put(name, arr):
        o, w = PP_OFF[name]
        pp[:, :, o:o + w] = arr[:, None, :]
    put("ln1_g", g("ln1_g")); put("ln1_b", g("ln1_b")); put("ln2_g", g("ln2_g")); put("ln2_b", g("ln2_b"))
    put("ssd_nw", g("ssd_norm_w")); put("dskip", g("ssd_d"))
    put("hg_nw", g("hg_norm_w")); put("ml_nw", g("ml_norm_w"))
    put("dt_bias", g("ssd_dt_bias").reshape(DEPTH, 32)); put("a_log", g("ssd_a_log").reshape(DEPTH, 32))
    put("ig_bias", g("ml_ig_bias").reshape(DEPTH, 8)); put("fg_bias", g("ml_fg_bias").reshape(DEPTH, 8))
    cw = np.concatenate([g("ssd_conv_w"), g("ml_conv_w")], axis=2)
    cb = np.concatenate([g("ssd_conv_b"), g("ml_conv_b")], axis=1)
    pc = np.zeros((DEPTH, 128, 120), np.float32)
    pc[:, :, 0:100] = cw.reshape(DEPTH, 5, 20, 128).transpose(0, 3, 2, 1).reshape(DEPTH, 128, 100)
    pc[:, :, 100:120] = cb.reshape(DEPTH, 20, 128).transpose(0, 2, 1)
    lbl = g("hg_lb_logits")
    lbtm = np.ascontiguousarray(np.broadcast_to(lbl[None], (128, DEPTH, 512)))
    lbfm = np.ascontiguousarray(lbl.reshape(DEPTH, 4, 128).transpose(2, 0, 1))
    win, wout = g("w_in"), g("w_out")
    f1, f3, f2 = g("ffn_w1"), g("ffn_w3"), g("ffn_w2")
    m1, m3, m2 = g("moe_w1"), g("moe_w3"), g("moe_w2")
    router = g("moe_router")
    router_l = np.ascontiguousarray(router.reshape(2, 8, 128, 8).transpose(0, 2, 1, 3).reshape(2, 128, 64))
    maps = []
    cmask_np = np.zeros((NCORES, 128, 16), np.float32)
    for k in range(NCORES):
        for j in range(NCORES):
            if j // 4 == k // 4 and j < k:
                cmask_np[k, :, j] = 1.0
            if j // 4 == k // 4 and j > k:
                cmask_np[k, :, 8 + j] = 1.0
    for k in range(NCORES):
        sel = np.zeros((32, 4), np.float32)
        if k % 4 != 0:
            sel[(k - 1) * 4 + 2, 0] = 1.0
            sel[(k - 1) * 4 + 3, 1] = 1.0
        if k % 4 != 3:
            sel[(k + 1) * 4 + 0, 2] = 1.0
            sel[(k + 1) * 4 + 1, 3] = 1.0
        maps.append({
            "x": x[k], "sel": sel, "cmask": cmask_np[k], "consts": consts, "lnin": lnin, "pp": pp, "pc": pc,
            "lbtm": lbtm, "lbfm": lbfm,
            "win_s": np.ascontiguousarray(win[:, k * 128:(k + 1) * 128, :]),
            "wout_s": np.ascontiguousarray(wout[:, k * 256:(k + 1) * 256, :]),
            "f1_s": np.ascontiguousarray(f1[:, k * 128:(k + 1) * 128, :]),
            "f3_s": np.ascontiguousarray(f3[:, k * 128:(k + 1) * 128, :]),
            "f2_s": np.ascontiguousarray(f2[:, k * 352:(k + 1) * 352, :]),
            "m1_s": np.ascontiguousarray(m1[:, k]), "m3_s": np.ascontiguousarray(m3[:, k]),
            "m2_s": np.ascontiguousarray(m2[:, k]), "router": router_l,
        })
    if n_layers < 2:
        for m in maps:
            for kk in ("m1_s", "m3_s", "m2_s", "router"):
                m.pop(kk)
    return maps


RUN_LAYERS = DEPTH
STOP_AFTER = None
DEBUG_DUMP = False
B1_PARTS = ("ssd", "ml", "hg")


def kernel(**inputs):
    nc, kb, L = build(RUN_LAYERS)
    finish(nc, kb, L)
    maps = make_inputs(inputs, RUN_LAYERS)
    res = run_bass_kernel_spmd(nc, maps, core_ids=list(range(NCORES)))
    out = np.stack([np.asarray(r["y"], dtype=np.float32) for r in res.results], axis=0)
    return out.reshape(2, 8192, D)
```
